# Optimizing a Trainium2 kernel written in Bass

```python
import math
import jax, jax.numpy as jnp
from jax import lax
import numpy as np

D_MODEL = 1024
BATCH = 8
SEQ = 4096
DEPTH = 1

HEAD_DIM = 64
N_HEADS_A = 8
DILATED_PATTERNS = ((128, 1), (512, 4), (2048, 16))
N_HEADS_B = 4
DIFF_V_DIM = 2 * HEAD_DIM
WIDTH_A = N_HEADS_A * HEAD_DIM
WIDTH_B = N_HEADS_B * DIFF_V_DIM
N_ALIBI_HEADS = N_HEADS_A + N_HEADS_B
ALIBI_IDX_A = (0, 1, 3, 4, 6, 7, 9, 10)
ALIBI_IDX_B = (2, 5, 8, 11)
IN_SIZES = (WIDTH_A, WIDTH_A, WIDTH_A,
            N_HEADS_B * 2 * HEAD_DIM, N_HEADS_B * 2 * HEAD_DIM, WIDTH_B,
            D_MODEL, D_MODEL)
IN_TOTAL = sum(IN_SIZES)
Q_BLOCK = 128
MASK_VALUE = -1e30
N_EXPERTS = 32
TOP_K = 4
D_EXPERT = D_MODEL
SWIGLU_ALPHA = 1.702
SWIGLU_LIMIT = 7.0
MOE_BLOCK = 256
LN_EPS = 1e-5
SUBLN_EPS = 1e-5
DEEPNORM_ALPHA = (2.0 * DEPTH) ** 0.25
DEEPNORM_BETA = (8.0 * DEPTH) ** -0.25

kernel_name = "hybrid_dilated_diffattn_moe_encoder"


def alibi_slopes(n):
    return jnp.asarray(2.0 ** (-8.0 * np.arange(1, n + 1) / n), jnp.float32)


def layer_norm(x, g, b):
    xf = x.astype(jnp.float32)
    mu = jnp.mean(xf, axis=-1, keepdims=True)
    var = jnp.mean(jnp.square(xf - mu), axis=-1, keepdims=True)
    y = (xf - mu) * lax.rsqrt(var + LN_EPS) * g.astype(jnp.float32) + b.astype(jnp.float32)
    return y.astype(x.dtype)


def dilated_window_attention(q, k, v, slopes, window, dilation):
    b, h, s, dh = q.shape
    half = window // (2 * dilation)
    L = s // dilation
    nb = -(-L // Q_BLOCK)
    Lp = nb * Q_BLOCK
    band = Q_BLOCK + 2 * half

    def to_residue(t):
        return t.reshape(b, h, L, dilation, dh).transpose(0, 1, 3, 2, 4)

    qr, kr, vr = to_residue(q), to_residue(k), to_residue(v)
    qb = jnp.pad(qr, ((0, 0), (0, 0), (0, 0), (0, Lp - L), (0, 0))).reshape(b, h, dilation, nb, Q_BLOCK, dh)
    kv_pad = ((0, 0), (0, 0), (0, 0), (half, Lp - L + half), (0, 0))
    kp, vp = jnp.pad(kr, kv_pad), jnp.pad(vr, kv_pad)
    idx = jnp.arange(nb)[:, None] * Q_BLOCK + jnp.arange(band)[None, :]
    kb = kp[:, :, :, idx]
    vb = vp[:, :, :, idx]

    scores = jnp.einsum('bhrnqe,bhrnke->bhrnqk', qb, kb).astype(jnp.float32) * (dh ** -0.5)
    rel = jnp.arange(Q_BLOCK)[:, None] - jnp.arange(band)[None, :] + half
    key_pos = idx - half
    valid = ((jnp.abs(rel) <= half)[None]
             & (key_pos[:, None, :] >= 0) & (key_pos[:, None, :] < L))
    bias = -slopes[:, None, None] * (dilation * jnp.abs(rel)).astype(jnp.float32)
    scores = jnp.where(valid, scores + bias[None, :, None, None], MASK_VALUE)
    m = jnp.max(scores, axis=-1, keepdims=True)
    p = jnp.exp(scores - m)
    z = jnp.sum(p, axis=-1, keepdims=True)
    o = jnp.einsum('bhrnqk,bhrnke->bhrnqe', (p / z).astype(v.dtype), vb)
    lse = (m + jnp.log(z))[..., 0]
    o = o.reshape(b, h, dilation, Lp, dh)[:, :, :, :L].transpose(0, 1, 3, 2, 4).reshape(b, h, s, dh)
    lse = lse.reshape(b, h, dilation, Lp)[..., :L].transpose(0, 1, 3, 2).reshape(b, h, s)
    return o, lse


def differential_attention(q1, q2, k1, k2, v, slopes, lam):
    b, h, s, dh = q1.shape
    nb = s // Q_BLOCK
    scale = dh ** -0.5
    key_pos = jnp.arange(s)

    def blocks(t):
        return t.reshape(b, h, nb, Q_BLOCK, dh).transpose(2, 0, 1, 3, 4)

    def one_block(args):
        q1b, q2b, start = args
        qpos = start + jnp.arange(Q_BLOCK)
        bias = -slopes[:, None, None] * jnp.abs(qpos[:, None] - key_pos[None, :]).astype(jnp.float32)
        a1 = jax.nn.softmax(jnp.einsum('bhqe,bhke->bhqk', q1b, k1).astype(jnp.float32) * scale + bias, axis=-1)
        a2 = jax.nn.softmax(jnp.einsum('bhqe,bhke->bhqk', q2b, k2).astype(jnp.float32) * scale + bias, axis=-1)
        return jnp.einsum('bhqk,bhkv->bhqv', (a1 - lam * a2).astype(v.dtype), v)

    starts = jnp.arange(nb, dtype=jnp.int32) * Q_BLOCK
    o = lax.map(one_block, (blocks(q1), blocks(q2), starts))
    return o.transpose(1, 2, 0, 3, 4).reshape(b, h, s, v.shape[-1])


def token_mixer(x, w_in, b_in, lambda_q1, lambda_k1, lambda_q2, lambda_k2, subln_g,
                w_proj_a, w_proj_b, w_out, b_out, lambda_init):
    bsz, seq, _ = x.shape
    proj = x @ w_in + b_in
    splits = [int(c) for c in np.cumsum(IN_SIZES)[:-1]]
    q_a, k_a, v_a, q_b, k_b, v_b, g_a, g_b = jnp.split(proj, splits, axis=-1)
    slopes = alibi_slopes(N_ALIBI_HEADS)

    def split_heads(t, n, dh):
        return t.reshape(bsz, seq, n, dh).transpose(0, 2, 1, 3)

    qa = split_heads(q_a, N_HEADS_A, HEAD_DIM)
    ka = split_heads(k_a, N_HEADS_A, HEAD_DIM)
    va = split_heads(v_a, N_HEADS_A, HEAD_DIM)
    slopes_a = slopes[np.array(ALIBI_IDX_A)]
    outs, lses = [], []
    for window, dilation in DILATED_PATTERNS:
        o, l = dilated_window_attention(qa, ka, va, slopes_a, window, dilation)
        outs.append(o)
        lses.append(l)
    mix_w = jax.nn.softmax(jnp.stack(lses), axis=0)
    o_a = jnp.sum(mix_w[..., None] * jnp.stack(outs).astype(jnp.float32), axis=0).astype(x.dtype)
    o_a = o_a.transpose(0, 2, 1, 3).reshape(bsz, seq, WIDTH_A)

    def split_pair(t):
        t = t.reshape(bsz, seq, N_HEADS_B, 2, HEAD_DIM).transpose(0, 2, 3, 1, 4)
        return t[:, :, 0], t[:, :, 1]

    q1, q2 = split_pair(q_b)
    k1, k2 = split_pair(k_b)
    vb = split_heads(v_b, N_HEADS_B, DIFF_V_DIM)
    lam = (jnp.exp(jnp.sum(lambda_q1.astype(jnp.float32) * lambda_k1.astype(jnp.float32)))
           - jnp.exp(jnp.sum(lambda_q2.astype(jnp.float32) * lambda_k2.astype(jnp.float32)))
           + lambda_init)
    o_b = differential_attention(q1, q2, k1, k2, vb, slopes[np.array(ALIBI_IDX_B)], lam)
    of = o_b.astype(jnp.float32)
    of = of * lax.rsqrt(jnp.mean(jnp.square(of), axis=-1, keepdims=True) + SUBLN_EPS)
    of = of * subln_g.astype(jnp.float32) * (1.0 - lambda_init)
    o_b = of.astype(x.dtype).transpose(0, 2, 1, 3).reshape(bsz, seq, WIDTH_B)

    merged = jax.nn.sigmoid(g_a) * (o_a @ w_proj_a) + jax.nn.sigmoid(g_b) * (o_b @ w_proj_b)
    return merged @ w_out + b_out


def moe_ffn(x, w_router, b_router, w_up, b_up, w_down, b_down):
    bsz, seq, d = x.shape
    xt = x.reshape(bsz * seq, d)
    n_tok = xt.shape[0]
    logits = (xt @ w_router + b_router).astype(jnp.float32)
    top_vals, top_idx = lax.top_k(logits, TOP_K)
    gates = jax.nn.softmax(top_vals, axis=-1).astype(x.dtype)

    n_assign = n_tok * TOP_K
    flat_e = top_idx.reshape(n_assign)
    flat_tok = jnp.arange(n_assign, dtype=jnp.int32) // TOP_K
    flat_w = gates.reshape(n_assign)
    order = jnp.argsort(flat_e)
    sorted_e = flat_e[order]
    counts = jnp.bincount(flat_e, length=N_EXPERTS)
    start = jnp.cumsum(counts) - counts
    padded_counts = (counts + MOE_BLOCK - 1) // MOE_BLOCK * MOE_BLOCK
    padded_end = jnp.cumsum(padded_counts)
    padded_start = padded_end - padded_counts
    dest = padded_start[sorted_e] + (jnp.arange(n_assign) - start[sorted_e])

    n_blocks = -(-n_assign // MOE_BLOCK) + N_EXPERTS
    n_rows = n_blocks * MOE_BLOCK
    buf_tok = jnp.zeros((n_rows,), jnp.int32).at[dest].set(flat_tok[order])
    buf_w = jnp.zeros((n_rows,), x.dtype).at[dest].set(flat_w[order])
    block_start = jnp.arange(n_blocks) * MOE_BLOCK
    block_e = jnp.minimum(jnp.searchsorted(padded_end, block_start, side='right'), N_EXPERTS - 1)

    def expert_block(args):
        tok, e = args
        xb = xt[tok]
        hcat = xb @ w_up[e] + b_up[e]
        gate = jnp.minimum(hcat[:, :D_EXPERT], SWIGLU_LIMIT)
        up = jnp.clip(hcat[:, D_EXPERT:], -SWIGLU_LIMIT, SWIGLU_LIMIT)
        act = gate * jax.nn.sigmoid(SWIGLU_ALPHA * gate) * (up + 1.0)
        return act @ w_down[e] + b_down[e]

    ys = lax.map(expert_block, (buf_tok.reshape(n_blocks, MOE_BLOCK), block_e))
    ys = ys.reshape(n_rows, d) * buf_w[:, None]
    out = jnp.zeros((n_tok, d), x.dtype).at[buf_tok].add(ys)
    return out.reshape(bsz, seq, d)


def setup_inputs(seed: int = 0) -> dict:
    key = jax.random.key(seed)
    ks = jax.random.split(key, 24)
    f32 = jnp.float32

    def nrm(k, shape, scale):
        return jax.random.normal(k, shape, f32) * scale

    col_scale = np.ones((IN_TOTAL,), np.float32)
    offs = np.concatenate([[0], np.cumsum(IN_SIZES)])
    col_scale[offs[2]:offs[3]] = DEEPNORM_BETA
    col_scale[offs[5]:offs[6]] = DEEPNORM_BETA

    return {
        "x": jax.random.normal(ks[0], (BATCH, SEQ, D_MODEL), f32),
        "w_in": nrm(ks[1], (DEPTH, D_MODEL, IN_TOTAL), D_MODEL ** -0.5) * jnp.asarray(col_scale),
        "b_in": nrm(ks[2], (DEPTH, IN_TOTAL), 0.01),
        "lambda_q1": nrm(ks[3], (DEPTH, HEAD_DIM), 0.1),
        "lambda_k1": nrm(ks[4], (DEPTH, HEAD_DIM), 0.1),
        "lambda_q2": nrm(ks[5], (DEPTH, HEAD_DIM), 0.1),
        "lambda_k2": nrm(ks[6], (DEPTH, HEAD_DIM), 0.1),
        "subln_g": 1.0 + nrm(ks[7], (DEPTH, DIFF_V_DIM), 0.02),
        "w_proj_a": nrm(ks[8], (DEPTH, WIDTH_A, D_MODEL), WIDTH_A ** -0.5),
        "w_proj_b": nrm(ks[9], (DEPTH, WIDTH_B, D_MODEL), WIDTH_B ** -0.5),
        "w_out": nrm(ks[10], (DEPTH, D_MODEL, D_MODEL), D_MODEL ** -0.5 * DEEPNORM_BETA),
        "b_out": nrm(ks[11], (DEPTH, D_MODEL), 0.01),
        "ln1_g": 1.0 + nrm(ks[12], (DEPTH, D_MODEL), 0.02),
        "ln1_b": nrm(ks[13], (DEPTH, D_MODEL), 0.02),
        "w_router": nrm(ks[14], (DEPTH, D_MODEL, N_EXPERTS), D_MODEL ** -0.5),
        "b_router": nrm(ks[15], (DEPTH, N_EXPERTS), 0.01),
        "w_up": nrm(ks[16], (DEPTH, N_EXPERTS, D_MODEL, 2 * D_EXPERT), D_MODEL ** -0.5),
        "b_up": nrm(ks[17], (DEPTH, N_EXPERTS, 2 * D_EXPERT), 0.01),
        "w_down": nrm(ks[18], (DEPTH, N_EXPERTS, D_EXPERT, D_MODEL), D_EXPERT ** -0.5 * DEEPNORM_BETA),
        "b_down": nrm(ks[19], (DEPTH, N_EXPERTS, D_MODEL), 0.01),
        "ln2_g": 1.0 + nrm(ks[20], (DEPTH, D_MODEL), 0.02),
        "ln2_b": nrm(ks[21], (DEPTH, D_MODEL), 0.02),
    }


def reference(x, w_in, b_in, lambda_q1, lambda_k1, lambda_q2, lambda_k2, subln_g,
              w_proj_a, w_proj_b, w_out, b_out, ln1_g, ln1_b,
              w_router, b_router, w_up, b_up, w_down, b_down, ln2_g, ln2_b):
    for layer in range(DEPTH):
        lambda_init = 0.8 - 0.6 * math.exp(-0.3 * layer)
        y = token_mixer(x, w_in[layer], b_in[layer], lambda_q1[layer], lambda_k1[layer],
                        lambda_q2[layer], lambda_k2[layer], subln_g[layer],
                        w_proj_a[layer], w_proj_b[layer], w_out[layer], b_out[layer], lambda_init)
        x = layer_norm(DEEPNORM_ALPHA * x + y, ln1_g[layer], ln1_b[layer])
        y = moe_ffn(x, w_router[layer], b_router[layer], w_up[layer], b_up[layer],
                    w_down[layer], b_down[layer])
        x = layer_norm(DEEPNORM_ALPHA * x + y, ln2_g[layer], ln2_b[layer])
    return x
```

```python
import numpy as np
import ml_dtypes
from contextlib import ExitStack
import concourse.bass as bass
import concourse.mybir as mybir
from concourse.bass_utils import run_bass_kernel_spmd

F32 = mybir.dt.float32
BF16 = mybir.dt.bfloat16
I32 = mybir.dt.int32
AF = mybir.ActivationFunctionType
ALU = mybir.AluOpType
AX = mybir.AxisListType
bf16 = ml_dtypes.bfloat16

S = 4096
D = 1024
NT = 32
CAP = 640
NE = 32
TRASH = NE * CAP
XROWS = NE * CAP + 128
ALPHA = 2.0 ** 0.25
LAMBDA_INIT = 0.2
LN_EPS = 1e-5
SLOPES = 2.0 ** (-8.0 * np.arange(1, 13) / 12)
IDX_A = (0, 1, 3, 4, 6, 7, 9, 10)
IDX_B = (2, 5, 8, 11)
DEFER_A = True
DEFER_B = False
GB = 1024
GA = 512

ENGS = ("pe", "act", "dve", "pool", "sp")


class Sem:
    def __init__(self, h):
        self.h = h
        self.count = 0


class Buf:
    __slots__ = ("name", "wc", "wd", "rc", "rd", "prc", "prd", "dsem")

    def __init__(self, name):
        self.name = name
        self.wc = {}
        self.wd = []
        self.rc = {}
        self.rd = []
        self.prc = {}
        self.prd = []
        self.dsem = None


class Op:
    __slots__ = ("eng", "fn", "deps", "signal", "count", "is_dma", "dsem", "dval")

    def __init__(self, eng, fn, is_dma):
        self.eng = eng
        self.fn = fn
        self.deps = []
        self.signal = False
        self.count = 0
        self.is_dma = is_dma
        self.dsem = None
        self.dval = 0


class Prog:
    def __init__(self, nc, es):
        self.nc = nc
        self.es = es
        self.streams = {e: [] for e in ENGS}
        self.msem = {e: Sem(es.enter_context(nc.semaphore("m_" + e))) for e in ENGS}
        self.free_sems = {}
        self.all_dsems = []
        self.nbuf = 0

    def buf(self, name=None):
        self.nbuf += 1
        return Buf(name or ("b%d" % self.nbuf))

    def get_dsem(self, kind):
        fl = self.free_sems.setdefault(kind, [])
        if fl:
            return fl.pop()
        s = Sem(self.es.enter_context(self.nc.semaphore("d%d" % len(self.all_dsems))))
        s.kind = kind
        self.all_dsems.append(s)
        return s

    def release(self, bufs):
        for b in bufs:
            if b.dsem is not None:
                self.free_sems.setdefault(b.dsem.kind, []).append(b.dsem)
                b.dsem = None

    def op(self, eng, fn, reads=(), writes=(), dma=None, disjoint=False, nowar=False):
        o = Op(eng, fn, dma is not None)
        deps = []
        for b in reads:
            for w in b.wc.values():
                deps.append((w, True))
            for w in b.wd:
                deps.append((w, True))
        for b in writes:
            if nowar:
                continue
            if b.rc or b.rd:
                b.prc, b.prd = b.rc, b.rd
                b.rc, b.rd = {}, []
                if disjoint:
                    b.wc, b.wd = {}, []
            for r in b.prc.values():
                deps.append((r, False))
            for r in b.prd:
                deps.append((r, False))
            if not disjoint:
                for w in b.wc.values():
                    deps.append((w, False))
                for w in b.wd:
                    deps.append((w, False))
                b.wc, b.wd = {}, []
        for (d, raw) in deps:
            if d is o:
                continue
            if (not d.is_dma) and (not o.is_dma) and d.eng == eng:
                if eng == "pe" or not raw:
                    continue
            o.deps.append(d)
        for b in writes:
            if o.is_dma:
                b.wd.append(o)
            else:
                b.wc[eng] = o
        for b in reads:
            if b in writes:
                continue
            if o.is_dma:
                b.rd.append(o)
            else:
                b.rc[eng] = o
        if o.is_dma:
            if dma.dsem is None:
                dma.dsem = self.get_dsem(eng)
            assert dma.dsem.kind == eng, (dma.name, eng)
            o.dsem = dma.dsem
            dma.dsem.count += 16
            o.dval = dma.dsem.count
        self.streams[eng].append(o)
        return o

    def barrier(self):
        lasts = []
        for e in ENGS:
            for x in reversed(self.streams[e]):
                if x.fn is not None:
                    lasts.append(x)
                    break
        dmas = [(s, s.count) for s in self.all_dsems if s.count > 0]
        for e in ENGS:
            o = Op(e, None, False)
            o.deps = [x for x in lasts if not (x.eng == e and not x.is_dma)]
            o.count = -1
            o.dval = dmas
            self.streams[e].append(o)

    def finalize(self):
        for e in ENGS:
            for o in self.streams[e]:
                for d in o.deps:
                    if not d.is_dma:
                        d.signal = True
        for e in ENGS:
            c = 0
            for o in self.streams[e]:
                if o.fn is None:
                    continue
                if o.signal and not o.is_dma:
                    c += 1
                    o.count = c
        nc = self.nc
        block = self.es.enter_context(nc.Block())

        def run(ename, engobj):
            waited = {}

            def wait(sem, val):
                if waited.get(id(sem), 0) >= val:
                    return
                waited[id(sem)] = val
                engobj.wait_ge(sem.h, val)

            for o in self.streams[ename]:
                need = {}
                for d in o.deps:
                    if d.is_dma:
                        s, v = d.dsem, d.dval
                    else:
                        s, v = self.msem[d.eng], d.count
                    if need.get(id(s), (None, 0))[1] < v:
                        need[id(s)] = (s, v)
                if o.fn is None:
                    for (s, v) in o.dval:
                        if need.get(id(s), (None, 0))[1] < v:
                            need[id(s)] = (s, v)
                for (s, v) in need.values():
                    wait(s, v)
                if o.fn is None:
                    continue
                ins = o.fn(engobj)
                if o.is_dma:
                    ins.then_inc(o.dsem.h, 16)
                elif o.signal:
                    ins.then_inc(self.msem[ename].h, 1)

        @block.tensor
        def _(e):
            run("pe", e)

        @block.scalar
        def _(e):
            run("act", e)

        @block.vector
        def _(e):
            run("dve", e)

        @block.gpsimd
        def _(e):
            run("pool", e)

        @block.sync
        def _(e):
            run("sp", e)


def host_consts():
    c = {}
    c["ident_bf"] = np.eye(128, dtype=np.float32).astype(bf16)
    c["ident_f"] = np.eye(128, dtype=np.float32)
    lt = (np.arange(128)[:, None] < np.arange(128)[None, :]).astype(np.float32)
    c["ltri"] = lt.astype(bf16)
    c["ones_bf"] = np.ones((128, 128), np.float32).astype(bf16)
    onesrow = np.zeros((128, 128), np.float32)
    onesrow[0, :] = 1.0
    c["onesrow"] = onesrow.astype(bf16)
    c["ecolm"] = np.tile((np.arange(NE, dtype=np.float32) * CAP - TRASH)[None, :], (128, 1)).astype(np.float32)
    j = np.arange(128)[:, None].astype(np.float64)
    x = np.arange(GB + GB - 128)[None, :].astype(np.float64)
    tzb = np.stack([np.exp(-SLOPES[i] * np.abs(x - j - (GB - 128))) for i in IDX_B])
    c["tzb"] = tzb.astype(np.float32).astype(bf16)
    xa = np.arange(GA + 19 * 128)[None, :].astype(np.float64)
    dl = xa - j - 1408
    ad = np.abs(dl)
    mult = (ad <= 64).astype(np.float64) + ((np.mod(dl, 4) == 0) & (ad <= 256)) + ((np.mod(dl, 16) == 0) & (ad <= 1024))
    tza = np.stack([mult * np.exp(-SLOPES[i] * ad) for i in IDX_A])
    c["tza"] = tza.astype(np.float32).astype(bf16)
    tl = np.arange(S) % GB
    hi = (tl // 32) * 32
    lo = tl % 32
    qaug = np.zeros((2, 2, S), np.float32)
    qaug[0, 0] = hi
    qaug[0, 1] = lo
    qaug[1, 0] = -hi
    qaug[1, 1] = -lo
    c["qaug"] = qaug.astype(bf16)
    kaug = np.zeros((4, 2, S), np.float32)
    for h, i in enumerate(IDX_B):
        kaug[h] = -8.0 * SLOPES[i]
    c["kaug"] = kaug.astype(bf16)
    ab = np.zeros((128, 4, 63), np.float32)
    p = np.arange(128, dtype=np.float64)
    for h, i in enumerate(IDX_B):
        for r in range(63):
            rel = r - 31
            if rel < 0:
                ab[:, h, r] = SLOPES[i] * (p + 128 * rel)
            else:
                ab[:, h, r] = -SLOPES[i] * (128 * rel + p)
    c["abias"] = ab.reshape(128, 4 * 63)
    c["mhalf"] = np.full((128, 8), -0.5, np.float32)
    return c


CONST_SPECS = None


def build(debug=None):
    debug = debug or {}
    last_phase = debug.get("last_phase", 5)
    nc = bass.Bass("TRN2", target_bir_lowering=False)
    consts = host_consts()

    def din(name, shape, dt):
        return nc.dram_tensor(name, list(shape), dt, kind="ExternalInput")

    xT = din("xT", [D, S], F32)
    xtok = din("x", [S, D], F32)
    w_in = din("w_in", [D, 5120], F32)
    w_pa = din("w_proj_a", [512, D], F32)
    w_pb = din("w_proj_b", [512, D], F32)
    w_out = din("w_out", [D, D], F32)
    w_router = din("w_router", [D, NE], F32)
    w_up = din("w_up", [NE, D, 2 * D], F32)
    w_down = din("w_down", [NE, D, D], F32)
    b_down = din("b_down", [NE, D], F32)
    bqk_d = din("bqk", [128, 16], F32)
    bv_d = din("bv_bc", [128, 1024], F32)
    bg_d = din("bg", [128, 16], F32)
    bout_d = din("bout_bc", [128, D], F32)
    ln1g_d = din("ln1g_bc", [128, D], F32)
    ln1b_d = din("ln1b_bc", [128, D], F32)
    ln2g_d = din("ln2g_bc", [128, D], F32)
    ln2b_d = din("ln2b_bc", [128, D], F32)
    subg_d = din("subg_bc", [128, 128], F32)
    lam_d = din("lam_bc", [128, 256], F32)
    brt_d = din("brouter_bc", [128, NE], F32)
    bupT_d = din("b_upT", [128, NE * 16], F32)
    cd = {}
    for k, v in consts.items():
        dt = BF16 if v.dtype == bf16 else F32
        cd[k] = din("c_" + k, v.shape, dt)
    out_d = nc.dram_tensor("out", [S, D], F32, kind="ExternalOutput")

    dbg_outs = debug.get("outs", ())

    def scr(name, shape, dt):
        if name in dbg_outs:
            return nc.dram_tensor(name, list(shape), dt, kind="ExternalOutput")
        return nc.dram_tensor(name, list(shape), dt)

    QTa = scr("QTa", [512, S], BF16)
    KTa = scr("KTa", [512, S], BF16)
    QTb = scr("QTb", [512, S], BF16)
    KTb = scr("KTb", [512, S], BF16)
    VA = scr("VA", [S, 8 * 65], BF16)
    VB = scr("VB", [S, 4 * 129], BF16)
    OT = scr("OT", [D, S], BF16)
    X1 = scr("X1", [S, D], F32)
    XG = scr("XG", [XROWS, D], BF16)
    YY = scr("YY", [XROWS, D], BF16)

    with ExitStack() as es:
        P = Prog(nc, es)

        def sbt(scope, name, shape, dt):
            t = scope.enter_context(nc.sbuf_tensor("s_" + name, list(shape), dt))
            return t, P.buf(name)

        def pst(scope, name, shape, dt):
            t = scope.enter_context(nc.psum_tensor("p_" + name, list(shape), dt))
            return t, P.buf(name)

        def dma(eng, out, in_, sbuf_buf, reads=(), writes=(), disjoint=False):
            return P.op(eng, lambda e: e.dma_start(out=out, in_=in_), reads=reads, writes=writes,
                        dma=sbuf_buf, disjoint=disjoint)

        def load(eng, t, b, src, disjoint=False):
            return dma(eng, t, src, b, writes=(b,), disjoint=disjoint)

        def store(eng, dst, t, b):
            return dma(eng, dst, t, b, reads=(b,))

        ident_bf, ident_bf_b = sbt(es, "ident_bf", [128, 128], BF16)
        ident_f, ident_f_b = sbt(es, "ident_f", [128, 128], F32)
        gates_all, gates_b = sbt(es, "gates_all", [128, NT, 4], F32)
        idx_all, idx_b = sbt(es, "idx_all", [128, NT, 4], I32)
        load("sp", ident_bf[:], ident_bf_b, cd["ident_bf"].ap())
        load("sp", ident_f[:], ident_f_b, cd["ident_f"].ap())

        def phase1():
          with ExitStack() as ph:
            wqkv, wqkv_b = sbt(ph, "wqkv", [128, 8, 3072], BF16)
            xg = [sbt(ph, "xg%d" % i, [128, 8, 512], BF16) for i in range(2)]
            bqk, bqk_b = sbt(ph, "bqk", [128, 16], F32)
            bvbc, bvbc_b = sbt(ph, "bvbc", [128, 1024], F32)
            stg = [sbt(ph, "stg%d" % i, [128, 16, 512], BF16) for i in range(2)]
            vas = [sbt(ph, "vas%d" % i, [128, 8, 65], BF16) for i in range(4)]
            vbs = [sbt(ph, "vbs%d" % i, [128, 4, 129], BF16) for i in range(4)]
            pss = [pst(ph, "p1ps%d" % i, [128, 512], F32) for i in range(6)]
            w_in_r = w_in.ap().rearrange("(kc p) n -> p kc n", p=128)
            for hf in range(2):
                load("pool", wqkv[:, :, hf * 1536:(hf + 1) * 1536], wqkv_b,
                     w_in_r[:, :, hf * 1536:(hf + 1) * 1536], disjoint=True)
            load("sp", bqk[:], bqk_b, bqk_d.ap())
            load("sp", bvbc[:], bvbc_b, bv_d.ap())
            for i in range(4):
                P.op("pool", lambda e, t=vas[i][0]: e.memset(t[:, :, 64:65], 1.0), writes=(vas[i][1],))
                P.op("pool", lambda e, t=vbs[i][0]: e.memset(t[:, :, 128:129], 1.0), writes=(vbs[i][1],))
            qk_tiles = []
            for i in range(4):
                qk_tiles.append((128 * i, QTa, i))
            for i in range(4):
                qk_tiles.append((512 + 128 * i, KTa, i))
            for i in range(4):
                qk_tiles.append((1536 + 128 * i, QTb, i))
            for i in range(4):
                qk_tiles.append((2048 + 128 * i, KTb, i))
            xT_r = xT.ap().rearrange("(kc p) t -> p kc t", p=128)
            pi = 0
            for tg in range(8):
                xgt, xgb = xg[tg % 2]
                load("pool", xgt[:], xgb, xT_r[:, :, tg * 512:(tg + 1) * 512])
                st, stb = stg[tg % 2]
                for mi, (c0, dst, di) in enumerate(qk_tiles):
                    ps, psb = pss[pi % 6]
                    pi += 1
                    for kc in range(8):
                        P.op("pe", lambda e, ps=ps, kc=kc, c0=c0, xgt=xgt: e.matmul(
                            ps[:], lhsT=wqkv[:, kc, c0:c0 + 128], rhs=xgt[:, kc, :], start=(kc == 0), stop=(kc == 7)),
                            reads=(wqkv_b, xgb), writes=(psb,))
                    if mi % 2 == 0:
                        P.op("act", lambda e, ps=ps, st=st, mi=mi: e.activation(
                            out=st[:, mi, :], in_=ps[:], func=AF.Identity, bias=bqk[:, mi:mi + 1], scale=1.0),
                            reads=(psb, bqk_b), writes=(stb,), disjoint=True)
                    else:
                        P.op("dve", lambda e, ps=ps, st=st, mi=mi: e.tensor_scalar(
                            out=st[:, mi, :], in0=ps[:], scalar1=bqk[:, mi:mi + 1], scalar2=None, op0=ALU.add),
                            reads=(psb, bqk_b), writes=(stb,), disjoint=True)
                for gi, dst in enumerate((QTa, KTa, QTb, KTb)):
                    store("sp", dst.ap()[:, tg * 512:(tg + 1) * 512].rearrange("(m p) t -> p m t", p=128),
                          st[:, gi * 4:(gi + 1) * 4, :], stb)
                for tt in range(4):
                    T = tg * 4 + tt
                    for which in range(2):
                        ps, psb = pss[pi % 6]
                        pi += 1
                        c0 = 1024 if which == 0 else 2560
                        for kc in range(8):
                            P.op("pe", lambda e, ps=ps, kc=kc, c0=c0, xgt=xgt, tt=tt: e.matmul(
                                ps[:], lhsT=xgt[:, kc, tt * 128:(tt + 1) * 128], rhs=wqkv[:, kc, c0:c0 + 512],
                                start=(kc == 0), stop=(kc == 7)), reads=(wqkv_b, xgb), writes=(psb,))
                        if which == 0:
                            vt, vb_ = vas[T % 4]
                            P.op("dve", lambda e, ps=ps, vt=vt: e.tensor_tensor(
                                out=vt[:, :, 0:64], in0=ps[:].rearrange("p (h d) -> p h d", d=64),
                                in1=bvbc[:, 0:512].rearrange("p (h d) -> p h d", d=64), op=ALU.add),
                                reads=(psb, bvbc_b), writes=(vb_,), disjoint=True)
                            store("sp", VA.ap()[T * 128:(T + 1) * 128, :], vt[:].rearrange("p h c -> p (h c)"), vb_)
                        else:
                            vt, vb_ = vbs[T % 4]
                            P.op("dve", lambda e, ps=ps, vt=vt: e.tensor_tensor(
                                out=vt[:, :, 0:128], in0=ps[:].rearrange("p (h d) -> p h d", d=128),
                                in1=bvbc[:, 512:1024].rearrange("p (h d) -> p h d", d=128), op=ALU.add),
                                reads=(psb, bvbc_b), writes=(vb_,), disjoint=True)
                            store("sp", VB.ap()[T * 128:(T + 1) * 128, :], vt[:].rearrange("p h c -> p (h c)"), vb_)
            P.barrier()
            P.release([wqkv_b, bqk_b, bvbc_b] + [b for _, b in xg + stg + vas + vbs])

        def phase2a():
          with ExitStack() as ph:
            Kt = [[sbt(ph, "Kt%d%d" % (s_, u), [128, S], BF16) for u in range(2)] for s_ in range(2)]
            Qb = [[sbt(ph, "Qb%d%d" % (s_, u), [128, S], BF16) for u in range(2)] for s_ in range(2)]
            Qa = [[sbt(ph, "Qa%d%d" % (s_, u), [128, S], BF16) for u in range(2)] for s_ in range(2)]
            Vh = [sbt(ph, "Vh%d" % s_, [128, NT, 129], BF16) for s_ in range(2)]
            tzb = [sbt(ph, "tzb%d" % s_, [128, 1920], BF16) for s_ in range(2)]
            abias, abias_b = sbt(ph, "abias", [128, 4 * 63], F32)
            subg, subg_b = sbt(ph, "subg", [128, 128], F32)
            lamin, lamin_b = sbt(ph, "lamin", [128, 256], F32)
            lamt, lamt_b = sbt(ph, "lamt", [128, 8], F32)
            mhalf, mhalf_b = sbt(ph, "mhalf", [128, 8], F32)
            junk, junk_b = sbt(ph, "junk", [128, 128], F32)
            PT = [sbt(ph, "PT%d" % i, [128, GB], BF16) for i in range(4)]
            o1, o1_b = sbt(ph, "o1", [128, 8, 128], F32)
            of, of_b = sbt(ph, "of", [128, 8, 128], F32)
            obf, obf_b = sbt(ph, "obf", [128, 8, 128], BF16)
            rz, rz_b = sbt(ph, "rz", [128, 32], F32)
            otst = [sbt(ph, "otst%d" % i, [128, 512], BF16) for i in range(2)]
            accsb, accsb_b = sbt(ph, "accsb", [128, 3, 480], F32)
            ST = [pst(ph, "ST%d" % i, [128, GB], F32) for i in range(2)]
            acc, acc_b = pst(ph, "acc", [128, 1536], F32)
            tp, tp_b = pst(ph, "tp2", [128, 512], BF16)

            def acc_ap(qt, c0, c1):
                base = (qt // 3) * 512 + (qt % 3) * 160
                return acc[:, base + c0:base + c1]

            load("sp", abias[:], abias_b, cd["abias"].ap())
            load("sp", subg[:], subg_b, subg_d.ap())
            load("sp", lamin[:], lamin_b, lam_d.ap())
            load("sp", mhalf[:], mhalf_b, cd["mhalf"].ap())
            for s_ in range(2):
                for u in range(2):
                    for (t, b) in (Kt[s_][u], Qb[s_][u], Qa[s_][u]):
                        P.op("pool", lambda e, t=t: e.memset(t[64:126, :], 0.0), writes=(b,))
            P.op("dve", lambda e: e.scalar_tensor_tensor(out=junk[:, 0:64], in0=lamin[:, 0:64], scalar=1.0, in1=lamin[:, 64:128],
                                                         op0=ALU.mult, op1=ALU.mult, accum_out=lamt[:, 0:1]),
                 reads=(lamin_b,), writes=(junk_b, lamt_b))
            P.op("dve", lambda e: e.scalar_tensor_tensor(out=junk[:, 64:128], in0=lamin[:, 128:192], scalar=1.0, in1=lamin[:, 192:256],
                                                         op0=ALU.mult, op1=ALU.mult, accum_out=lamt[:, 1:2]),
                 reads=(lamin_b, lamt_b), writes=(junk_b, lamt_b))
            P.op("act", lambda e: e.activation(out=lamt[:, 2:4], in_=lamt[:, 0:2], func=AF.Exp), reads=(lamt_b,), writes=(lamt_b,))
            P.op("dve", lambda e: e.tensor_tensor(out=lamt[:, 4:5], in0=lamt[:, 3:4], in1=lamt[:, 2:3], op=ALU.subtract),
                 reads=(lamt_b,), writes=(lamt_b,))
            P.op("dve", lambda e: e.tensor_scalar(out=lamt[:, 4:5], in0=lamt[:, 4:5], scalar1=-LAMBDA_INIT, scalar2=None, op0=ALU.add),
                 reads=(lamt_b,), writes=(lamt_b,))
            neglam = lamt[:, 4:5]

            def load_head(h):
                s_ = h % 2
                for u in range(2):
                    r0 = (2 * h + u) * 64
                    for (t, b), src, aug in ((Kt[s_][u], KTb, cd["kaug"].ap()[h]),
                                             (Qb[s_][u], QTb, cd["qaug"].ap()[0]),
                                             (Qa[s_][u], QTb, cd["qaug"].ap()[1])):
                        load("sp", t[0:64, :], b, src.ap()[r0:r0 + 64, :], disjoint=True)
                        load("sp", t[126:128, :], b, aug, disjoint=True)
                load("sp", Vh[s_][0][:], Vh[s_][1], VB.ap()[:, h * 129:(h + 1) * 129].rearrange("(k p) c -> p k c", p=128))
                load("sp", tzb[s_][0][:], tzb[s_][1], cd["tzb"].ap()[h])

            c_rs = 1.0 / ((1.0 - LAMBDA_INIT) ** 2)
            SUBLN_EPS = 1e-5
            steps = []
            for h in range(4):
                for g in range(4):
                    for u in range(2):
                        for kb in range(NT):
                            steps.append((h, g, u, kb))
            load_head(0)
            ot_i = [0]
            deferred = []
            cur_i = [0]

            def sacc(qt, c0, c1):
                return accsb[:, qt // 3, (qt % 3) * 160 + c0:(qt % 3) * 160 + c1]

            def finalize_unit(h, g, u):
                q0 = g * GB
                for bk in range(3):
                    w = 480 if bk < 2 else 320
                    P.op("dve", lambda e, bk=bk, w=w: e.tensor_copy(
                        out=accsb[:, bk, 0:w].rearrange("p (a c) -> p a c", c=160)[:, :, 0:129],
                        in_=acc[:, bk * 512:bk * 512 + w].rearrange("p (a c) -> p a c", c=160)[:, :, 0:129]),
                         reads=(acc_b,), writes=(accsb_b,), disjoint=(bk > 0))
                if u == 0:
                    for qt in range(8):
                        P.op("dve", lambda e, qt=qt: e.reciprocal(out=rz[:, qt:qt + 1], in_=sacc(qt, 128, 129)),
                             reads=(accsb_b,), writes=(rz_b,), disjoint=True)
                        P.op("dve", lambda e, qt=qt: e.tensor_scalar(out=o1[:, qt, :], in0=sacc(qt, 0, 128), scalar1=rz[:, qt:qt + 1],
                                                                      scalar2=None, op0=ALU.mult),
                             reads=(accsb_b, rz_b), writes=(o1_b,), disjoint=True)
                    return
                for qt in range(8):
                    P.op("dve", lambda e, qt=qt: e.reciprocal(out=rz[:, 8 + qt:9 + qt], in_=sacc(qt, 128, 129)),
                         reads=(accsb_b,), writes=(rz_b,), disjoint=True)
                    P.op("dve", lambda e, qt=qt: e.tensor_scalar(out=rz[:, 16 + qt:17 + qt], in0=rz[:, 8 + qt:9 + qt], scalar1=neglam,
                                                                  scalar2=None, op0=ALU.mult),
                         reads=(rz_b, lamt_b), writes=(rz_b,))
                    P.op("dve", lambda e, qt=qt: e.scalar_tensor_tensor(out=of[:, qt, :], in0=sacc(qt, 0, 128), scalar=rz[:, 16 + qt:17 + qt],
                                                                         in1=o1[:, qt, :], op0=ALU.mult, op1=ALU.add),
                         reads=(accsb_b, rz_b, o1_b), writes=(of_b,), disjoint=True)
                    P.op("dve", lambda e, qt=qt: e.scalar_tensor_tensor(out=junk[:], in0=of[:, qt, :], scalar=1.0, in1=of[:, qt, :],
                                                                         op0=ALU.mult, op1=ALU.mult, accum_out=rz[:, 24 + qt:25 + qt]),
                         reads=(of_b, rz_b), writes=(junk_b, rz_b))
                P.op("dve", lambda e: e.tensor_scalar(out=rz[:, 8:16], in0=rz[:, 24:32], scalar1=c_rs / 128.0, scalar2=SUBLN_EPS * c_rs,
                                                      op0=ALU.mult, op1=ALU.add), reads=(rz_b,), writes=(rz_b,))
                P.op("pool", lambda e: e.tensor_tensor(out=rz[:, 16:24], in0=rz[:, 8:16], in1=mhalf[:, 0:8], op=ALU.pow),
                     reads=(rz_b, mhalf_b), writes=(rz_b,))
                for qt in range(8):
                    P.op("dve", lambda e, qt=qt: e.scalar_tensor_tensor(out=obf[:, qt, :], in0=of[:, qt, :], scalar=rz[:, 16 + qt:17 + qt],
                                                                         in1=subg[:], op0=ALU.mult, op1=ALU.mult),
                         reads=(of_b, rz_b, subg_b), writes=(obf_b,), disjoint=True)
                def partB(h=h, q0=q0):
                    for half in range(2):
                        for j in range(4):
                            qt = half * 4 + j
                            P.op("pe", lambda e, qt=qt, j=j: e.transpose(tp[:, j * 128:(j + 1) * 128], obf[:, qt, :], ident_bf[:]),
                                 reads=(obf_b, ident_bf_b), writes=(tp_b,), disjoint=(j > 0))
                        ott, otb = otst[ot_i[0] % 2]
                        ot_i[0] += 1
                        P.op("dve", lambda e, ott=ott: e.tensor_copy(out=ott[:], in_=tp[:]), reads=(tp_b,), writes=(otb,))
                        store("sp", OT.ap()[512 + h * 128:512 + (h + 1) * 128, q0 + half * 512:q0 + (half + 1) * 512], ott[:], otb)
                if DEFER_A:
                    deferred.append([cur_i[0] + 26, partB])
                else:
                    partB()

            def emit_pv(i):
                h, g, u, kb = steps[i]
                s_ = h % 2
                ptt, ptb = PT[i % 4]
                vt, vb_ = Vh[s_]
                for qt in range(8):
                    first = (kb == 0 and qt % 3 == 0)
                    P.op("pe", lambda e, qt=qt, kb=kb, ptt=ptt, vt=vt, first=first: e.matmul(
                        acc_ap(qt, 0, 129), lhsT=ptt[:, qt * 128:(qt + 1) * 128], rhs=vt[:, kb, :],
                        start=first, stop=(kb == NT - 1), skip_group_check=True),
                        reads=(ptb, vb_), writes=(acc_b,), disjoint=(not (kb == 0 and qt == 0)))
                if kb == NT - 1:
                    finalize_unit(h, g, u)

            n = len(steps)
            for i in range(n + 2):
                if i < n:
                    h, g, u, kb = steps[i]
                    s_ = h % 2
                    q0 = g * GB
                    k0 = kb * 128
                    rel = kb - g * (GB // 128)
                    (kt_t, kt_b) = Kt[s_][u]
                    if rel < 0:
                        (q_t, q_b), kr = Qb[s_][u], 128
                    elif rel >= GB // 128:
                        (q_t, q_b), kr = Qa[s_][u], 128
                    else:
                        (q_t, q_b), kr = Qb[s_][u], 126
                    stt, stb_ = ST[i % 2]
                    for c in range(GB // 512):
                        P.op("pe", lambda e, stt=stt, kt_t=kt_t, q_t=q_t, kr=kr, k0=k0, q0=q0, c=c: e.matmul(
                            stt[:, c * 512:(c + 1) * 512], lhsT=kt_t[0:kr, k0:k0 + 128], rhs=q_t[0:kr, q0 + c * 512:q0 + (c + 1) * 512],
                            start=True, stop=True), reads=(kt_b, q_b), writes=(stb_,), disjoint=(c > 0))
                    ptt, ptb = PT[i % 4]
                    if 0 <= rel < GB // 128:
                        P.op("act", lambda e, ptt=ptt, stt=stt: e.activation(out=ptt[:], in_=stt[:], func=AF.Exp, scale=0.125),
                             reads=(stb_,), writes=(ptb,))
                        woff = (GB - 128) - 128 * rel
                        tzt, tzb_ = tzb[s_]
                        P.op("dve", lambda e, ptt=ptt, tzt=tzt, woff=woff: e.tensor_tensor(
                            out=ptt[:], in0=ptt[:], in1=tzt[:, woff:woff + GB], op=ALU.mult),
                            reads=(ptb, tzb_), writes=(ptb,))
                    else:
                        col = h * 63 + rel + 31
                        P.op("act", lambda e, ptt=ptt, stt=stt, col=col: e.activation(
                            out=ptt[:], in_=stt[:], func=AF.Exp, bias=abias[:, col:col + 1], scale=0.125),
                            reads=(stb_, abias_b), writes=(ptb,))
                cur_i[0] = i
                if i >= 2:
                    emit_pv(i - 2)
                while deferred and deferred[0][0] <= i:
                    deferred.pop(0)[1]()
                if 1 <= i <= n:
                    h, g, u, kb = steps[i - 1]
                    if g == 0 and u == 0 and kb == 0 and h + 1 < 4:
                        load_head(h + 1)
            while deferred:
                deferred.pop(0)[1]()
            P.barrier()
            P.release([b for pair in Kt + Qb + Qa for (_, b) in pair] + [b for _, b in Vh + tzb + PT + otst] +
                      [abias_b, subg_b, lamin_b, mhalf_b])

        def phase2b():
          with ExitStack() as ph:
            Qh = [sbt(ph, "Qh%d" % s_, [128, S], BF16) for s_ in range(2)]
            Kh = [sbt(ph, "Kh%d" % s_, [128, S], BF16) for s_ in range(2)]
            tza = [sbt(ph, "tza%d" % s_, [128, 2944], BF16) for s_ in range(2)]
            Va, Va_b = sbt(ph, "Va", [128, NT, 520], BF16)
            PT = [sbt(ph, "PTa%d" % i, [128, GA], BF16) for i in range(5)]
            oabs = [sbt(ph, "oab%d" % i, [128, 4, 128], BF16) for i in range(2)]
            rz, rz_b = sbt(ph, "rza", [128, 4], F32)
            otst = [sbt(ph, "otsa%d" % i, [64, 512], BF16) for i in range(2)]
            ST = [pst(ph, "STa%d" % i, [128, GA], F32) for i in range(4)]
            accs = [pst(ph, "acca%d" % i, [128, 512], F32) for i in range(2)]
            tp, tp_b = pst(ph, "tp2a", [128, 512], BF16)
            load("sp", Va[:], Va_b, VA.ap().rearrange("(k p) c -> p k c", p=128))
            for (t_, b_) in oabs:
                P.op("pool", lambda e, t_=t_: e.memset(t_[:], 0.0), writes=(b_,))
            for s_ in range(2):
                P.op("pool", lambda e, t=Qh[s_][0]: e.memset(t[64:128, :], 0.0), writes=(Qh[s_][1],))
                P.op("pool", lambda e, t=Kh[s_][0]: e.memset(t[64:128, :], 0.0), writes=(Kh[s_][1],))

            def load_head_a(h):
                s_ = h % 2
                load("sp", Qh[s_][0][0:64, :], Qh[s_][1], QTa.ap()[h * 64:(h + 1) * 64, :], disjoint=True)
                load("sp", Kh[s_][0][0:64, :], Kh[s_][1], KTa.ap()[h * 64:(h + 1) * 64, :], disjoint=True)
                load("sp", tza[s_][0][:], tza[s_][1], cd["tza"].ap()[h])

            steps = []
            for h in range(8):
                for g in range(8):
                    kbs = [kb for kb in range(4 * g - 8, 4 * g + 12) if 0 <= kb < NT]
                    for j, kb in enumerate(kbs):
                        steps.append((h, g, kb, j == 0, j == len(kbs) - 1))
            load_head_a(0)
            ot_i = [0]
            unit_i = [0]
            deferred = []
            cur_i = [0]

            def acc_of(ui, qt, c0, c1):
                a, ab_ = accs[ui % 2]
                return a[:, qt * 80 + c0:qt * 80 + c1], ab_

            def finalize_a(h, g, ui):
                q0 = g * GA
                oab, oab_b = oabs[ui % 2]
                for qt in range(4):
                    zap, ab_ = acc_of(ui, qt, 64, 65)
                    uap, _ = acc_of(ui, qt, 0, 64)
                    P.op("dve", lambda e, qt=qt, zap=zap: e.reciprocal(out=rz[:, qt:qt + 1], in_=zap),
                         reads=(ab_,), writes=(rz_b,), disjoint=True)
                    P.op("dve", lambda e, qt=qt, uap=uap: e.tensor_scalar(out=oab[:, qt, 0:64], in0=uap, scalar1=rz[:, qt:qt + 1],
                                                                          scalar2=None, op0=ALU.mult),
                         reads=(ab_, rz_b), writes=(oab_b,), disjoint=True)
                def partB(h=h, q0=q0, oab=oab, oab_b=oab_b):
                    for qt in range(4):
                        P.op("pe", lambda e, qt=qt: e.transpose(tp[:, qt * 128:(qt + 1) * 128], oab[:, qt, :], ident_bf[:]),
                             reads=(oab_b, ident_bf_b), writes=(tp_b,), disjoint=(qt > 0))
                    ott, otb = otst[ot_i[0] % 2]
                    ot_i[0] += 1
                    P.op("dve", lambda e, ott=ott: e.tensor_copy(out=ott[:], in_=tp[0:64, :]), reads=(tp_b,), writes=(otb,))
                    store("sp", OT.ap()[h * 64:(h + 1) * 64, q0:q0 + GA], ott[:], otb)
                if DEFER_B:
                    deferred.append([cur_i[0] + 9, partB])
                else:
                    partB()

            def emit_pv_a(i, ui):
                h, g, kb, first_kb, last_kb = steps[i]
                ptt, ptb = PT[i % 5]
                for qt in range(4):
                    oap, ab_ = acc_of(ui, qt, 0, 65)
                    P.op("pe", lambda e, qt=qt, kb=kb, ptt=ptt, oap=oap, first=(first_kb and qt == 0), h=h, last_kb=last_kb: e.matmul(
                        oap, lhsT=ptt[:, qt * 128:(qt + 1) * 128], rhs=Va[:, kb, h * 65:(h + 1) * 65],
                        start=first, stop=last_kb, skip_group_check=True),
                        reads=(ptb, Va_b), writes=(ab_,), disjoint=(not (first_kb and qt == 0)))
                if last_kb:
                    finalize_a(h, g, ui)

            n = len(steps)
            uis = []
            ui = -1
            for i in range(n):
                if steps[i][3]:
                    ui += 1
                uis.append(ui)
            for i in range(n + 3):
                if i < n:
                    h, g, kb, first_kb, last_kb = steps[i]
                    s_ = h % 2
                    if g == 0 and first_kb and h + 1 < 8:
                        load_head_a(h + 1)
                    q0 = g * GA
                    k0 = kb * 128
                    stt, stb_ = ST[i % 4]
                    P.op("pe", lambda e, stt=stt, kt=Kh[s_][0], qt_=Qh[s_][0], k0=k0, q0=q0: e.matmul(
                        stt[:], lhsT=kt[:, k0:k0 + 128], rhs=qt_[:, q0:q0 + GA], start=True, stop=True),
                        reads=(Kh[s_][1], Qh[s_][1]), writes=(stb_,))
                    ptt, ptb = PT[i % 5]
                    P.op("act", lambda e, ptt=ptt, stt=stt: e.activation(out=ptt[:], in_=stt[:], func=AF.Exp, scale=0.125),
                         reads=(stb_,), writes=(ptb,))
                    b_ = kb - 4 * g + 8
                    woff = 2432 - 128 * b_
                    tzt, tzb_ = tza[s_]
                    meng = "dve"
                    P.op(meng, lambda e, ptt=ptt, tzt=tzt, woff=woff: e.tensor_tensor(
                        out=ptt[:], in0=ptt[:], in1=tzt[:, woff:woff + GA], op=ALU.mult),
                        reads=(ptb, tzb_), writes=(ptb,))
                cur_i[0] = i
                if i >= 3:
                    emit_pv_a(i - 3, uis[i - 3])
                while deferred and deferred[0][0] <= i:
                    deferred.pop(0)[1]()
            while deferred:
                deferred.pop(0)[1]()
            P.barrier()
            P.release([b for _, b in Qh + Kh + tza + PT + otst + oabs] + [Va_b])

        def phase3():
          with ExitStack() as ph:
            wg, wg_b = sbt(ph, "wg", [128, 8, 2048], BF16)
            wpa, wpa_b = sbt(ph, "wpa", [128, 4, 1024], BF16)
            wpb, wpb_b = sbt(ph, "wpb", [128, 4, 1024], BF16)
            wout, wout_b = sbt(ph, "wout", [128, 8, 1024], BF16)
            wr, wr_b = sbt(ph, "wr", [128, 8, NE], F32)
            bg, bg_b = sbt(ph, "bg", [128, 16], F32)
            boutb, boutb_b = sbt(ph, "boutb", [128, D], F32)
            l1g, l1g_b = sbt(ph, "l1g", [128, D], F32)
            l1b, l1b_b = sbt(ph, "l1b", [128, D], F32)
            brt, brt_b = sbt(ph, "brt", [128, NE], F32)
            ltri, ltri_b = sbt(ph, "ltri", [128, 128], BF16)
            onesb, onesb_b = sbt(ph, "onesb", [128, 128], BF16)
            ecolm, ecolm_b = sbt(ph, "ecolm", [128, NE], F32)
            mhalf, mhalf_b = sbt(ph, "mhalf3", [128, 8], F32)
            cum, cum_b = sbt(ph, "cum", [128, NE], BF16)
            onesrow3, onesrow3_b = sbt(ph, "onesrow3", [128, 128], BF16)
            boutp, boutp_b = sbt(ph, "boutp", [128, D], BF16)
            xg = [sbt(ph, "xg3_%d" % i, [128, 8, 512], BF16) for i in range(2)]
            otg = [sbt(ph, "otg%d" % i, [128, 8, 512], BF16) for i in range(2)]
            mT = [sbt(ph, "mT%d" % i, [128, 8, 512], BF16) for i in range(2)]
            sa = [sbt(ph, "sa%d" % i, [128, 512], F32) for i in range(2)]
            sb_ = [sbt(ph, "sb%d" % i, [128, 512], F32) for i in range(2)]
            t1 = [sbt(ph, "t1_%d" % i, [128, 512], F32) for i in range(2)]
            t2 = [sbt(ph, "t2_%d" % i, [128, 512], F32) for i in range(2)]
            xt_ = [sbt(ph, "xtk%d" % i, [128, D], F32) for i in range(2)]
            zt = [sbt(ph, "zt%d" % i, [128, D], F32) for i in range(2)]
            x1t = [sbt(ph, "x1t%d" % i, [128, D], F32) for i in range(4)]
            x1bf = [sbt(ph, "x1bf%d" % i, [128, D], BF16) for i in range(4)]
            x1T = [sbt(ph, "x1T%d" % i, [128, 8, 128], F32) for i in range(2)]
            sm = [sbt(ph, "sm%d" % i, [128, 400], F32) for i in range(3)]
            lns = [sbt(ph, "lns%d" % i, [128, 32], F32) for i in range(2)]
            mbf = [sbt(ph, "mbf%d" % i, [128, NE], BF16) for i in range(3)]
            pool4 = [pst(ph, "p3ps%d" % i, [128, 512], F32) for i in range(4)]
            yps = [pst(ph, "yps", [128, 1024], F32)]
            xTp = [pst(ph, "xTp", [128, 512], F32)]
            lgp = [pst(ph, "lgp", [128, 64], F32)]
            pos_b = P.buf("pos_b")
            w_in_r = w_in.ap().rearrange("(kc p) n -> p kc n", p=128)
            load("pool", wg[:], wg_b, w_in_r[:, :, 3072:5120])
            load("pool", wpa[:], wpa_b, w_pa.ap().rearrange("(kc p) n -> p kc n", p=128))
            load("pool", wpb[:], wpb_b, w_pb.ap().rearrange("(kc p) n -> p kc n", p=128))
            load("pool", wout[:], wout_b, w_out.ap().rearrange("(kc p) n -> p kc n", p=128))
            load("sp", wr[:], wr_b, w_router.ap().rearrange("(kc p) n -> p kc n", p=128))
            for (t, b, src) in ((bg, bg_b, bg_d), (boutb, boutb_b, bout_d), (l1g, l1g_b, ln1g_d), (l1b, l1b_b, ln1b_d),
                                (brt, brt_b, brt_d), (ltri, ltri_b, cd["ltri"]), (onesb, onesb_b, cd["ones_bf"]),
                                (ecolm, ecolm_b, cd["ecolm"]), (mhalf, mhalf_b, cd["mhalf"])):
                load("sp", t[:], b, src.ap())
            P.op("pool", lambda e: e.memset(cum[:], 0.0), writes=(cum_b,))
            P.op("pool", lambda e: e.memset(boutp[:], 0.0), writes=(boutp_b,))
            load("pool", boutp[0:1, :], boutp_b, bout_d.ap()[0:1, :])
            load("sp", onesrow3[:], onesrow3_b, cd["onesrow"].ap())
            xT_r = xT.ap().rearrange("(kc p) t -> p kc t", p=128)
            OT_r = OT.ap().rearrange("(kc p) t -> p kc t", p=128)
            pi = [0]

            def nextps():
                r = pool4[pi[0] % 4]
                pi[0] += 1
                return r

            yp, ypb = yps[0]
            xp, xpb = xTp[0]
            lp, lpb = lgp[0]

            def S0(T, mTt, mTb, tt):
                for hf in range(2):
                    P.op("pe", lambda e, hf=hf: e.matmul(yp[:, hf * 512:(hf + 1) * 512], lhsT=onesrow3[:], rhs=boutp[:, hf * 512:(hf + 1) * 512],
                                                       start=True, stop=False), reads=(onesrow3_b, boutp_b), writes=(ypb,), disjoint=(hf > 0))
                    for m in range(8):
                        P.op("pe", lambda e, m=m, hf=hf, tt=tt, mTt=mTt: e.matmul(
                            yp[:, hf * 512:(hf + 1) * 512], lhsT=mTt[:, m, tt * 128:(tt + 1) * 128], rhs=wout[:, m, hf * 512:(hf + 1) * 512],
                            start=False, stop=(m == 7)), reads=(mTb, wout_b), writes=(ypb,), disjoint=True)

            def S1(T):
                xk, xkb = xt_[T % 2]
                z, zb = zt[T % 2]
                x1, x1b = x1t[T % 4]
                xb16, xb16b = x1bf[T % 4]
                lt, ltb = lns[T % 2]
                load("sp", xk[:], xkb, xtok.ap()[T * 128:(T + 1) * 128, :])
                P.op("dve", lambda e: e.scalar_tensor_tensor(out=z[:], in0=xk[:], scalar=ALPHA, in1=yp[:], op0=ALU.mult, op1=ALU.add),
                     reads=(xkb, ypb), writes=(zb,))
                emit_ln(P, z, zb, x1, x1b, lt, ltb, l1g, l1g_b, l1b, l1b_b, mhalf, mhalf_b, o=0, use_act=True, aff_eng=("dve", "dve"))
                store("sp", X1.ap()[T * 128:(T + 1) * 128, :], x1[:], x1b)

            def S2(T):
                x1, x1b = x1t[T % 4]
                xTt, xTb = x1T[T % 2]
                for half in range(2):
                    for j in range(4):
                        kc = half * 4 + j
                        P.op("pe", lambda e, j=j, kc=kc: e.transpose(xp[:, j * 128:(j + 1) * 128], x1[:, kc * 128:(kc + 1) * 128], ident_f[:]),
                             reads=(x1b, ident_f_b), writes=(xpb,), disjoint=(j > 0))
                    P.op("act", lambda e, half=half: e.activation(
                        out=xTt[:, half * 4:(half + 1) * 4, :], in_=xp[:].rearrange("p (a b) -> p a b", b=128), func=AF.Identity),
                        reads=(xpb,), writes=(xTb,), disjoint=(half > 0))
                for kc in range(8):
                    P.op("pe", lambda e, kc=kc: e.matmul(lp[:, 0:NE], lhsT=xTt[:, kc, :], rhs=wr[:, kc, :], start=(kc == 0), stop=(kc == 7)),
                         reads=(xTb, wr_b), writes=(lpb,), disjoint=(kc > 0))

            def S3(T):
                smt, smb = sm[T % 3]
                mb, mbb = mbf[T % 3]
                x1, x1b = x1t[T % 4]
                xb16, xb16b = x1bf[T % 4]
                P.op("act", lambda e: e.activation(out=xb16[:], in_=x1[:], func=AF.Identity), reads=(x1b,), writes=(xb16b,))
                P.op("dve", lambda e: e.tensor_tensor(out=smt[:, 0:32], in0=lp[:, 0:NE], in1=brt[:], op=ALU.add),
                     reads=(lpb, brt_b), writes=(smb,))
                P.op("dve", lambda e: e.max(out=smt[:, 32:40], in_=smt[:, 0:32]), reads=(smb,), writes=(smb,))
                P.op("dve", lambda e: e.tensor_scalar(out=smt[:, 64:96], in0=smt[:, 0:32], scalar1=smt[:, 35:36], scalar2=None, op0=ALU.is_ge),
                     reads=(smb,), writes=(smb,))
                P.op("dve", lambda e: e.tensor_scalar(out=smt[:, 40:41], in0=smt[:, 32:33], scalar1=-1.0, scalar2=None, op0=ALU.mult),
                     reads=(smb,), writes=(smb,))
                P.op("act", lambda e: e.activation(out=smt[:, 96:128], in_=smt[:, 0:32], func=AF.Sigmoid, bias=smt[:, 40:41], scale=1.0),
                     reads=(smb,), writes=(smb,))
                P.op("act", lambda e: e.activation(out=smt[:, 128:160], in_=smt[:, 0:32], func=AF.Sigmoid, bias=smt[:, 32:33], scale=-1.0),
                     reads=(smb,), writes=(smb,))
                P.op("dve", lambda e: e.reciprocal(out=smt[:, 128:160], in_=smt[:, 128:160]), reads=(smb,), writes=(smb,))
                P.op("dve", lambda e: e.tensor_tensor(out=smt[:, 160:192], in0=smt[:, 96:128], in1=smt[:, 128:160], op=ALU.mult),
                     reads=(smb,), writes=(smb,))
                P.op("dve", lambda e: e.scalar_tensor_tensor(out=smt[:, 160:192], in0=smt[:, 160:192], scalar=1.0, in1=smt[:, 64:96],
                                                             op0=ALU.mult, op1=ALU.mult, accum_out=smt[:, 192:193]),
                     reads=(smb,), writes=(smb,))
                P.op("dve", lambda e: e.reciprocal(out=smt[:, 193:194], in_=smt[:, 192:193]), reads=(smb,), writes=(smb,))
                P.op("dve", lambda e: e.tensor_scalar(out=smt[:, 224:256], in0=smt[:, 160:192], scalar1=smt[:, 193:194], scalar2=None, op0=ALU.mult),
                     reads=(smb,), writes=(smb,))
                P.op("dve", lambda e: e.tensor_copy(out=mb[:], in_=smt[:, 64:96]), reads=(smb,), writes=(mbb,))

            def S4(T):
                mb, mbb = mbf[T % 3]
                P.op("pe", lambda e: e.matmul(lp[:, 32:64], lhsT=ltri[:], rhs=mb[:], start=True, stop=False, skip_group_check=True),
                     reads=(mbb, ltri_b), writes=(pos_b,))
                P.op("pe", lambda e: e.matmul(lp[:, 32:64], lhsT=onesb[:], rhs=cum[:], start=False, stop=True, skip_group_check=True),
                     reads=(cum_b, onesb_b), writes=(pos_b,), disjoint=True)

            def S5(T):
                smt, smb = sm[T % 3]
                mb, mbb = mbf[T % 3]
                xb16, xb16b = x1bf[T % 4]
                P.op("dve", lambda e: e.tensor_scalar(out=smt[:, 288:320], in0=lp[:, 32:64], scalar1=float(CAP), scalar2=None, op0=ALU.is_lt),
                     reads=(pos_b,), writes=(smb,))
                P.op("dve", lambda e: e.tensor_tensor(out=smt[:, 256:288], in0=lp[:, 32:64], in1=ecolm[:], op=ALU.add),
                     reads=(pos_b, ecolm_b, smb), writes=(smb,))
                P.op("pool", lambda e: e.tensor_tensor(out=cum[:], in0=cum[:], in1=mb[:], op=ALU.add), reads=(cum_b, mbb), writes=(cum_b,))
                P.op("dve", lambda e: e.tensor_tensor(out=smt[:, 256:288], in0=smt[:, 256:288], in1=smt[:, 288:320], op=ALU.mult),
                     reads=(smb,), writes=(smb,))
                for kk in range(4):
                    P.op("dve", lambda e, kk=kk: e.tensor_scalar(out=smt[:, 320:352], in0=smt[:, 0:32], scalar1=smt[:, 32 + kk:33 + kk],
                                                                 scalar2=None, op0=ALU.is_equal), reads=(smb,), writes=(smb,))
                    P.op("dve", lambda e, kk=kk: e.scalar_tensor_tensor(out=smt[:, 352:384], in0=smt[:, 320:352], scalar=1.0, in1=smt[:, 256:288],
                                                                        op0=ALU.mult, op1=ALU.mult, accum_out=smt[:, 384 + kk:385 + kk]),
                         reads=(smb,), writes=(smb,))
                    P.op("dve", lambda e, kk=kk: e.scalar_tensor_tensor(out=smt[:, 352:384], in0=smt[:, 320:352], scalar=1.0, in1=smt[:, 224:256],
                                                                        op0=ALU.mult, op1=ALU.mult, accum_out=gates_all[:, T, kk:kk + 1]),
                         reads=(smb,), writes=(smb,))
                P.op("dve", lambda e: e.tensor_scalar(out=idx_all[:, T, :], in0=smt[:, 384:388], scalar1=float(TRASH), scalar2=None, op0=ALU.add),
                     reads=(smb,), writes=(idx_b,), nowar=True)
                for kk in range(4):
                    P.op("pool", lambda e, kk=kk: e.indirect_dma_start(
                        out=XG.ap()[:, :], out_offset=bass.IndirectOffsetOnAxis(ap=idx_all[:, T, kk:kk + 1], axis=0),
                        in_=xb16[:, :], in_offset=None), reads=(xb16b, idx_b), dma=xb16b)

            def load_tg(tg):
                load("pool", xg[tg % 2][0][:], xg[tg % 2][1], xT_r[:, :, tg * 512:(tg + 1) * 512])
                load("sp", otg[tg % 2][0][:], otg[tg % 2][1], OT_r[:, :, tg * 512:(tg + 1) * 512])

            for tg in range(8):
                xgt, xgb = xg[tg % 2]
                ogt, ogb = otg[tg % 2]
                mTt, mTb = mT[tg % 2]
                load_tg(tg)
                for m in range(8):
                    k = (tg * 8 + m) % 2
                    ps, psb = nextps()
                    for kc in range(8):
                        P.op("pe", lambda e, ps=ps, kc=kc, m=m, xgt=xgt: e.matmul(ps[:], lhsT=wg[:, kc, m * 128:(m + 1) * 128], rhs=xgt[:, kc, :],
                                                                                 start=(kc == 0), stop=(kc == 7)),
                             reads=(wg_b, xgb), writes=(psb,))
                    P.op("act", lambda e, ps=ps, k=k, m=m: e.activation(out=sa[k][0][:], in_=ps[:], func=AF.Sigmoid, bias=bg[:, m:m + 1], scale=1.0),
                         reads=(psb, bg_b), writes=(sa[k][1],))
                    ps, psb = nextps()
                    for kc in range(4):
                        P.op("pe", lambda e, ps=ps, kc=kc, m=m, ogt=ogt: e.matmul(ps[:], lhsT=wpa[:, kc, m * 128:(m + 1) * 128], rhs=ogt[:, kc, :],
                                                                                 start=(kc == 0), stop=(kc == 3)),
                             reads=(wpa_b, ogb), writes=(psb,))
                    P.op("dve", lambda e, ps=ps, k=k: e.tensor_tensor(out=t1[k][0][:], in0=sa[k][0][:], in1=ps[:], op=ALU.mult),
                         reads=(psb, sa[k][1]), writes=(t1[k][1],))
                    ps, psb = nextps()
                    for kc in range(8):
                        P.op("pe", lambda e, ps=ps, kc=kc, m=m, xgt=xgt: e.matmul(ps[:], lhsT=wg[:, kc, 1024 + m * 128:1024 + (m + 1) * 128],
                                                                                 rhs=xgt[:, kc, :], start=(kc == 0), stop=(kc == 7)),
                             reads=(wg_b, xgb), writes=(psb,))
                    P.op("act", lambda e, ps=ps, k=k, m=m: e.activation(out=sb_[k][0][:], in_=ps[:], func=AF.Sigmoid, bias=bg[:, 8 + m:9 + m], scale=1.0),
                         reads=(psb, bg_b), writes=(sb_[k][1],))
                    ps, psb = nextps()
                    for kc in range(4):
                        P.op("pe", lambda e, ps=ps, kc=kc, m=m, ogt=ogt: e.matmul(ps[:], lhsT=wpb[:, kc, m * 128:(m + 1) * 128], rhs=ogt[:, 4 + kc, :],
                                                                                 start=(kc == 0), stop=(kc == 3)),
                             reads=(wpb_b, ogb), writes=(psb,))
                    P.op("dve", lambda e, ps=ps, k=k: e.tensor_tensor(out=t2[k][0][:], in0=sb_[k][0][:], in1=ps[:], op=ALU.mult),
                         reads=(psb, sb_[k][1]), writes=(t2[k][1],))
                    P.op("pool", lambda e, k=k, m=m, mTt=mTt: e.tensor_tensor(out=mTt[:, m, :], in0=t1[k][0][:], in1=t2[k][0][:], op=ALU.add),
                         reads=(t1[k][1], t2[k][1]), writes=(mTb,), disjoint=True)
                for tt in range(4):
                    T = tg * 4 + tt
                    S0(T, mTt, mTb, tt)
                    if T - 2 >= 0:
                        S2(T - 2)
                    if T - 3 >= 0:
                        S4(T - 3)
                    S1(T)
                    if T - 2 >= 0:
                        S3(T - 2)
                    if T - 3 >= 0:
                        S5(T - 3)
            for T in range(NT, NT + 3):
                if T - 2 < NT:
                    S2(T - 2)
                if T - 3 < NT:
                    S4(T - 3)
                if T - 2 < NT:
                    S3(T - 2)
                if T - 3 < NT:
                    S5(T - 3)
            P.barrier()
            rel = [onesrow3_b, boutp_b, wg_b, wpa_b, wpb_b, wout_b, wr_b, bg_b, boutb_b, l1g_b, l1b_b, brt_b, ltri_b, onesb_b, ecolm_b, mhalf_b]
            rel += [b for _, b in xg + otg + xt_ + x1t + x1bf]
            P.release(rel)

        def phase4():
          with ExitStack() as ph:
            wup = [sbt(ph, "wup%d" % i, [128, 8, 2048], BF16) for i in range(2)]
            wdn = [sbt(ph, "wdn%d" % i, [128, 8, 1024], BF16) for i in range(2)]
            bdp = [sbt(ph, "bdp%d" % i, [128, 1024], BF16) for i in range(2)]
            bup, bup_b = sbt(ph, "bup", [128, NE * 16], F32)
            onesrow, onesrow_b = sbt(ph, "onesrow", [128, 128], BF16)
            zero_t, zero_b = sbt(ph, "zero_t", [128, D], BF16)
            xgr = [sbt(ph, "xgr%d" % i, [128, 5, D], BF16) for i in range(2)]
            xgT = [sbt(ph, "xgT%d" % i, [128, 8, CAP], BF16) for i in range(2)]
            actT = [sbt(ph, "actT%d" % i, [128, 8, CAP], BF16) for i in range(2)]
            yst = [sbt(ph, "yst%d" % i, [128, 5, D], BF16) for i in range(2)]
            gp = [sbt(ph, "gp%d" % i, [128, 320], F32) for i in range(2)]
            sg = [sbt(ph, "sg%d" % i, [128, 320], F32) for i in range(2)]
            tu = [sbt(ph, "tu%d" % i, [128, 320], F32) for i in range(2)]
            gs = [sbt(ph, "gs%d" % i, [128, 320], F32) for i in range(2)]
            pg = [pst(ph, "pg%d" % i, [128, 512], F32) for i in range(2)]
            pu = [pst(ph, "pu%d" % i, [128, 512], F32) for i in range(2)]
            yph = [pst(ph, "yp4_%d" % i, [128, 512], F32) for i in range(2)]
            tps_ = [pst(ph, "tp4_%d" % i, [128, 512], BF16) for i in range(2)]
            tpi = [0]
            load("sp", bup[:], bup_b, bupT_d.ap())
            load("sp", onesrow[:], onesrow_b, cd["onesrow"].ap())
            P.op("dve", lambda e: e.tensor_scalar(out=bup[:].rearrange("p (e m) -> p e m", m=16)[:, :, 8:16],
                                                  in0=bup[:].rearrange("p (e m) -> p e m", m=16)[:, :, 8:16], scalar1=1.0, scalar2=None, op0=ALU.add),
                 reads=(bup_b,), writes=(bup_b,))
            P.op("pool", lambda e: e.memset(zero_t[:], 0.0), writes=(zero_b,))
            store("sp", YY.ap()[TRASH:TRASH + 128, :], zero_t[:], zero_b)
            for i in range(2):
                P.op("pool", lambda e, t=bdp[i][0]: e.memset(t[:], 0.0), writes=(bdp[i][1],))

            def load_w(e_):
                s_ = e_ % 2
                load("pool", wup[s_][0][:], wup[s_][1], w_up.ap()[e_].rearrange("(kc p) n -> p kc n", p=128))
                load("pool", wdn[s_][0][:], wdn[s_][1], w_down.ap()[e_].rearrange("(kc p) n -> p kc n", p=128))
                load("pool", bdp[s_][0][0:1, :], bdp[s_][1], b_down.ap()[e_:e_ + 1, :])
                for kc in range(8):
                    P.op("sp", lambda e, kc=kc, e_=e_, t_=xgT[s_][0]: e.dma_start_transpose(
                        out=t_[:, kc, :], in_=XG.ap()[e_ * CAP:(e_ + 1) * CAP, kc * 128:(kc + 1) * 128]),
                        writes=(xgT[s_][1],), dma=xgT[s_][1], disjoint=(kc > 0))

            def transposes(e_):
                return
                s_ = e_ % 2
                xr, xrb = xgr[s_]
                xT_, xTb = xgT[s_]
                for t in range(5):
                    for half in range(2):
                        tp, tp_b = tps_[tpi[0] % 2]
                        tpi[0] += 1
                        for j in range(4):
                            kc = half * 4 + j
                            P.op("pe", lambda e, j=j, kc=kc, t=t, xr=xr, tp=tp: e.transpose(tp[:, j * 128:(j + 1) * 128], xr[:, t, kc * 128:(kc + 1) * 128], ident_bf[:]),
                                 reads=(xrb, ident_bf_b), writes=(tp_b,), disjoint=(j > 0))
                        P.op("act", lambda e, half=half, t=t, xT_=xT_, tp=tp: e.activation(
                            out=xT_[:, half * 4:(half + 1) * 4, t * 128:(t + 1) * 128], in_=tp[:].rearrange("p (a b) -> p a b", b=128), func=AF.Identity),
                            reads=(tp_b,), writes=(xTb,), disjoint=True)

            def up(e_):
                s_ = e_ % 2
                wu, wub = wup[s_]
                xT_, xTb = xgT[s_]
                aT, aTb = actT[s_]
                it = 0
                for m in range(8):
                    for hf in range(2):
                        n0 = hf * 320
                        k = it % 2
                        it += 1
                        pgt, pgb = pg[k]
                        put, pub = pu[k]
                        for kc in range(8):
                            P.op("pe", lambda e, kc=kc, m=m, n0=n0, pgt=pgt, wu=wu, xT_=xT_: e.matmul(
                                pgt[:, 0:320], lhsT=wu[:, kc, m * 128:(m + 1) * 128], rhs=xT_[:, kc, n0:n0 + 320], start=(kc == 0), stop=(kc == 7)),
                                reads=(wub, xTb), writes=(pgb,))
                        for kc in range(8):
                            P.op("pe", lambda e, kc=kc, m=m, n0=n0, put=put, wu=wu, xT_=xT_: e.matmul(
                                put[:, 0:320], lhsT=wu[:, kc, 1024 + m * 128:1024 + (m + 1) * 128], rhs=xT_[:, kc, n0:n0 + 320], start=(kc == 0), stop=(kc == 7)),
                                reads=(wub, xTb), writes=(pub,))
                        cg = e_ * 16 + m
                        cu = e_ * 16 + 8 + m
                        P.op("dve", lambda e, k=k, pgt=pgt, cg=cg: e.tensor_scalar(out=gp[k][0][:], in0=pgt[:, 0:320], scalar1=bup[:, cg:cg + 1], scalar2=7.0,
                                                                                  op0=ALU.add, op1=ALU.min), reads=(pgb, bup_b), writes=(gp[k][1],))
                        P.op("act", lambda e, k=k: e.activation(out=sg[k][0][:], in_=gp[k][0][:], func=AF.Sigmoid, scale=1.702),
                             reads=(gp[k][1],), writes=(sg[k][1],))
                        P.op("dve", lambda e, k=k, put=put, cu=cu: e.tensor_scalar(out=tu[k][0][:], in0=put[:, 0:320], scalar1=bup[:, cu:cu + 1], scalar2=8.0,
                                                                                  op0=ALU.add, op1=ALU.min), reads=(pub, bup_b), writes=(tu[k][1],))
                        P.op("dve", lambda e, k=k: e.tensor_tensor(out=gs[k][0][:], in0=gp[k][0][:], in1=sg[k][0][:], op=ALU.mult),
                             reads=(gp[k][1], sg[k][1]), writes=(gs[k][1],))
                        P.op("dve", lambda e, k=k, m=m, n0=n0, aT=aT: e.scalar_tensor_tensor(out=aT[:, m, n0:n0 + 320], in0=tu[k][0][:], scalar=-6.0, in1=gs[k][0][:],
                                                                                            op0=ALU.max, op1=ALU.mult),
                             reads=(tu[k][1], gs[k][1]), writes=(aTb,), disjoint=True)

            def down(e_):
                s_ = e_ % 2
                wd, wdb = wdn[s_]
                aT, aTb = actT[s_]
                bd, bdb = bdp[s_]
                ys, ysb = yst[s_]
                for t in range(5):
                    for hf in range(2):
                        yp, ypb = yph[hf]
                        P.op("pe", lambda e, hf=hf, bd=bd, yp=yp: e.matmul(yp[:], lhsT=onesrow[:], rhs=bd[:, hf * 512:(hf + 1) * 512],
                                                                    start=True, stop=False), reads=(onesrow_b, bdb), writes=(ypb,))
                        for kc in range(8):
                            P.op("pe", lambda e, hf=hf, kc=kc, t=t, aT=aT, wd=wd, yp=yp: e.matmul(
                                yp[:], lhsT=aT[:, kc, t * 128:(t + 1) * 128], rhs=wd[:, kc, hf * 512:(hf + 1) * 512],
                                start=False, stop=(kc == 7)), reads=(aTb, wdb), writes=(ypb,), disjoint=True)
                        P.op("act", lambda e, t=t, ys=ys, hf=hf, yp=yp: e.activation(out=ys[:, t, hf * 512:(hf + 1) * 512], in_=yp[:], func=AF.Identity),
                             reads=(ypb,), writes=(ysb,), disjoint=True)
                store("sp", YY.ap()[e_ * CAP:(e_ + 1) * CAP, :].rearrange("(t p) d -> p t d", p=128), ys[:], ysb)

            load_w(0)
            load_w(1)
            transposes(0)
            for e_ in range(NE):
                up(e_)
                if e_ + 1 < NE:
                    transposes(e_ + 1)
                down(e_)
                if e_ + 2 < NE:
                    load_w(e_ + 2)
            P.barrier()
            P.release([b for _, b in wup + wdn + bdp + xgr + yst] + [bup_b, onesrow_b, zero_b])

        def phase5():
          with ExitStack() as ph:
            l2g, l2g_b = sbt(ph, "l2g", [128, D], F32)
            l2b, l2b_b = sbt(ph, "l2b", [128, D], F32)
            mhalf, mhalf_b = sbt(ph, "mhalf5", [128, 8], F32)
            yk = [sbt(ph, "yk%d" % i, [128, 4, D], BF16) for i in range(3)]
            x1l = [sbt(ph, "x1l%d" % i, [128, D], F32) for i in range(3)]
            ac = [sbt(ph, "ac%d" % i, [128, D], F32) for i in range(2)]
            ob = [sbt(ph, "ob%d" % i, [128, D], F32) for i in range(2)]
            sm = [sbt(ph, "sm5_%d" % i, [128, 32], F32) for i in range(2)]
            load("sp", l2g[:], l2g_b, ln2g_d.ap())
            load("sp", l2b[:], l2b_b, ln2b_d.ap())
            load("sp", mhalf[:], mhalf_b, cd["mhalf"].ap())
            def fetch5(T):
                ykt, ykb = yk[T % 3]
                xl, xlb = x1l[T % 3]
                for kk in range(4):
                    P.op("pool", lambda e, T=T, kk=kk, ykt=ykt: e.indirect_dma_start(
                        out=ykt[:, kk, :], out_offset=None, in_=YY.ap()[:, :],
                        in_offset=bass.IndirectOffsetOnAxis(ap=idx_all[:, T, kk:kk + 1], axis=0)),
                        reads=(idx_b,), writes=(ykb,), dma=ykb, disjoint=(kk > 0))
                load("sp", xl[:], xlb, X1.ap()[T * 128:(T + 1) * 128, :])

            def comp5(T):
                ykt, ykb = yk[T % 3]
                xl, xlb = x1l[T % 3]
                a, ab_ = ac[T % 2]
                o, ob_ = ob[T % 2]
                smt, smb = sm[T % 2]
                P.op("act", lambda e, T=T, ykt=ykt, a=a: e.activation(out=a[:], in_=ykt[:, 0, :], func=AF.Identity, scale=gates_all[:, T, 0:1]),
                     reads=(ykb, gates_b), writes=(ab_,))
                for kk in range(1, 4):
                    P.op("dve", lambda e, T=T, kk=kk, ykt=ykt, a=a: e.scalar_tensor_tensor(out=a[:], in0=ykt[:, kk, :], scalar=gates_all[:, T, kk:kk + 1], in1=a[:],
                                                                                       op0=ALU.mult, op1=ALU.add), reads=(ykb, gates_b, ab_), writes=(ab_,))
                P.op("dve", lambda e, xl=xl, a=a: e.scalar_tensor_tensor(out=a[:], in0=xl[:], scalar=ALPHA, in1=a[:], op0=ALU.mult, op1=ALU.add),
                     reads=(xlb, ab_), writes=(ab_,))
                emit_ln(P, a, ab_, o, ob_, smt, smb, l2g, l2g_b, l2b, l2b_b, mhalf, mhalf_b, use_act=True, aff_eng=("dve", "pool"))
                store("sp", out_d.ap()[T * 128:(T + 1) * 128, :], o[:], ob_)

            for T in range(NT + 2):
                if T < NT:
                    fetch5(T)
                if T >= 2:
                    comp5(T - 2)
            P.barrier()

        phase1()
        if last_phase >= 2:
            phase2a()
            phase2b()
        if last_phase >= 3:
            phase3()
        if last_phase >= 4:
            phase4()
        if last_phase >= 5:
            phase5()
        P.finalize()
    return nc


def emit_ln(P, z, zb, x1, x1b, smt, smb, g, g_b, b, b_b, mhalf, mhalf_b, o=0, use_act=False, aff_eng=("pool", "pool")):
    P.op("dve", lambda e: e.bn_stats(out=smt[:, o:o + 6], in_=z[:, 0:512]), reads=(zb,), writes=(smb,))
    P.op("dve", lambda e: e.bn_stats(out=smt[:, o + 6:o + 12], in_=z[:, 512:1024]), reads=(zb, smb), writes=(smb,))
    P.op("dve", lambda e: e.bn_aggr(out=smt[:, o + 12:o + 14], in_=smt[:, o:o + 12]), reads=(smb,), writes=(smb,))
    P.op("dve", lambda e: e.tensor_scalar(out=smt[:, o + 14:o + 15], in0=smt[:, o + 13:o + 14], scalar1=LN_EPS, scalar2=None, op0=ALU.add),
         reads=(smb,), writes=(smb,))
    P.op("pool", lambda e: e.tensor_tensor(out=smt[:, o + 15:o + 16], in0=smt[:, o + 14:o + 15], in1=mhalf[:, 0:1], op=ALU.pow),
         reads=(smb, mhalf_b), writes=(smb,))
    if use_act:
        P.op("dve", lambda e: e.tensor_scalar(out=smt[:, o + 16:o + 17], in0=smt[:, o + 12:o + 13], scalar1=smt[:, o + 15:o + 16], scalar2=-1.0,
                                              op0=ALU.mult, op1=ALU.mult), reads=(smb,), writes=(smb,))
        P.op("act", lambda e: e.activation(out=x1[:], in_=z[:], func=AF.Identity, bias=smt[:, o + 16:o + 17], scale=smt[:, o + 15:o + 16]),
             reads=(zb, smb), writes=(x1b,))
    else:
        P.op("dve", lambda e: e.tensor_scalar(out=x1[:], in0=z[:], scalar1=smt[:, o + 12:o + 13], scalar2=smt[:, o + 15:o + 16],
                                              op0=ALU.subtract, op1=ALU.mult), reads=(zb, smb), writes=(x1b,))
    P.op(aff_eng[0], lambda e: e.tensor_tensor(out=x1[:], in0=x1[:], in1=g[:], op=ALU.mult), reads=(x1b, g_b), writes=(x1b,))
    P.op(aff_eng[1], lambda e: e.tensor_tensor(out=x1[:], in0=x1[:], in1=b[:], op=ALU.add), reads=(x1b, b_b), writes=(x1b,))


_NC_CACHE = {}


def prep_shared(inp):
    f = lambda a: np.ascontiguousarray(np.asarray(a, dtype=np.float32))
    b_in = f(inp["b_in"])[0]
    sh = {}
    sh["w_in"] = f(inp["w_in"])[0]
    sh["w_proj_a"] = f(inp["w_proj_a"])[0]
    sh["w_proj_b"] = f(inp["w_proj_b"])[0]
    sh["w_out"] = f(inp["w_out"])[0]
    sh["w_router"] = f(inp["w_router"])[0]
    sh["w_up"] = f(inp["w_up"])[0]
    sh["w_down"] = f(inp["w_down"])[0]
    sh["b_down"] = f(inp["b_down"])[0]
    qkcols = np.concatenate([np.arange(0, 512), np.arange(512, 1024), np.arange(1536, 2048), np.arange(2048, 2560)])
    sh["bqk"] = np.ascontiguousarray(b_in[qkcols].reshape(16, 128).T)
    bv = np.concatenate([b_in[1024:1536], b_in[2560:3072]])
    sh["bv_bc"] = np.ascontiguousarray(np.tile(bv[None, :], (128, 1)))
    sh["bg"] = np.ascontiguousarray(b_in[3072:5120].reshape(16, 128).T)
    bc = lambda v: np.ascontiguousarray(np.tile(f(v).reshape(1, -1), (128, 1)))
    sh["bout_bc"] = bc(inp["b_out"][0])
    sh["ln1g_bc"] = bc(inp["ln1_g"][0])
    sh["ln1b_bc"] = bc(inp["ln1_b"][0])
    sh["ln2g_bc"] = bc(inp["ln2_g"][0])
    sh["ln2b_bc"] = bc(inp["ln2_b"][0])
    sh["subg_bc"] = bc(inp["subln_g"][0])
    lam = np.concatenate([f(inp["lambda_q1"])[0], f(inp["lambda_k1"])[0], f(inp["lambda_q2"])[0], f(inp["lambda_k2"])[0]])
    sh["lam_bc"] = bc(lam)
    sh["brouter_bc"] = bc(inp["b_router"][0])
    bup = f(inp["b_up"])[0]
    sh["b_upT"] = np.ascontiguousarray(bup.reshape(NE, 16, 128).transpose(2, 0, 1).reshape(128, NE * 16))
    for k, v in host_consts().items():
        sh["c_" + k] = np.ascontiguousarray(v)
    return sh


def kernel(**inputs):
    x = np.asarray(inputs["x"], dtype=np.float32)
    sh = prep_shared(inputs)
    if "nc" not in _NC_CACHE:
        _NC_CACHE["nc"] = build()
    nc = _NC_CACHE["nc"]
    in_maps = []
    for c in range(8):
        m = dict(sh)
        m["x"] = np.ascontiguousarray(x[c])
        m["xT"] = np.ascontiguousarray(x[c].T)
        in_maps.append(m)
    res = run_bass_kernel_spmd(nc, in_maps, core_ids=list(range(8)))
    out = np.stack([np.asarray(r["out"], dtype=np.float32) for r in res.results], axis=0)
    return out
```

```python
import numpy as np
import ml_dtypes
from contextlib import ExitStack
import concourse.bass as bass
import concourse.mybir as mybir
from concourse.bass_utils import run_bass_kernel_spmd

F32 = mybir.dt.float32
BF16 = mybir.dt.bfloat16
I32 = mybir.dt.int32
AF = mybir.ActivationFunctionType
ALU = mybir.AluOpType
AX = mybir.AxisListType
bf16 = ml_dtypes.bfloat16

S = 4096
D = 1024
NT = 32
CAP = 640
NE = 32
TRASH = NE * CAP
XROWS = NE * CAP + 128
ALPHA = 2.0 ** 0.25
LAMBDA_INIT = 0.2
LN_EPS = 1e-5
SLOPES = 2.0 ** (-8.0 * np.arange(1, 13) / 12)
IDX_A = (0, 1, 3, 4, 6, 7, 9, 10)
IDX_B = (2, 5, 8, 11)
DEFER_A = True
DEFER_B = False
GB = 1024
GA = 512

ENGS = ("pe", "act", "dve", "pool", "sp")


class Sem:
    def __init__(self, h):
        self.h = h
        self.count = 0


class Buf:
    __slots__ = ("name", "wc", "wd", "rc", "rd", "prc", "prd", "dsem")

    def __init__(self, name):
        self.name = name
        self.wc = {}
        self.wd = []
        self.rc = {}
        self.rd = []
        self.prc = {}
        self.prd = []
        self.dsem = None


class Op:
    __slots__ = ("eng", "fn", "deps", "signal", "count", "is_dma", "dsem", "dval")

    def __init__(self, eng, fn, is_dma):
        self.eng = eng
        self.fn = fn
        self.deps = []
        self.signal = False
        self.count = 0
        self.is_dma = is_dma
        self.dsem = None
        self.dval = 0


class Prog:
    def __init__(self, nc, es):
        self.nc = nc
        self.es = es
        self.streams = {e: [] for e in ENGS}
        self.msem = {e: Sem(es.enter_context(nc.semaphore("m_" + e))) for e in ENGS}
        self.free_sems = {}
        self.all_dsems = []
        self.nbuf = 0

    def buf(self, name=None):
        self.nbuf += 1
        return Buf(name or ("b%d" % self.nbuf))

    def get_dsem(self, kind):
        fl = self.free_sems.setdefault(kind, [])
        if fl:
            return fl.pop()
        s = Sem(self.es.enter_context(self.nc.semaphore("d%d" % len(self.all_dsems))))
        s.kind = kind
        self.all_dsems.append(s)
        return s

    def release(self, bufs):
        for b in bufs:
            if b.dsem is not None:
                self.free_sems.setdefault(b.dsem.kind, []).append(b.dsem)
                b.dsem = None

    def op(self, eng, fn, reads=(), writes=(), dma=None, disjoint=False, nowar=False):
        o = Op(eng, fn, dma is not None)
        deps = []
        for b in reads:
            for w in b.wc.values():
                deps.append((w, True))
            for w in b.wd:
                deps.append((w, True))
        for b in writes:
            if nowar:
                continue
            if b.rc or b.rd:
                b.prc, b.prd = b.rc, b.rd
                b.rc, b.rd = {}, []
                if disjoint:
                    b.wc, b.wd = {}, []
            for r in b.prc.values():
                deps.append((r, False))
            for r in b.prd:
                deps.append((r, False))
            if not disjoint:
                for w in b.wc.values():
                    deps.append((w, False))
                for w in b.wd:
                    deps.append((w, False))
                b.wc, b.wd = {}, []
        for (d, raw) in deps:
            if d is o:
                continue
            if (not d.is_dma) and (not o.is_dma) and d.eng == eng:
                if eng == "pe" or not raw:
                    continue
            o.deps.append(d)
        for b in writes:
            if o.is_dma:
                b.wd.append(o)
            else:
                b.wc[eng] = o
        for b in reads:
            if b in writes:
                continue
            if o.is_dma:
                b.rd.append(o)
            else:
                b.rc[eng] = o
        if o.is_dma:
            if dma.dsem is None:
                dma.dsem = self.get_dsem(eng)
            assert dma.dsem.kind == eng, (dma.name, eng)
            o.dsem = dma.dsem
            dma.dsem.count += 16
            o.dval = dma.dsem.count
        self.streams[eng].append(o)
        return o

    def barrier(self):
        lasts = []
        for e in ENGS:
            for x in reversed(self.streams[e]):
                if x.fn is not None:
                    lasts.append(x)
                    break
        dmas = [(s, s.count) for s in self.all_dsems if s.count > 0]
        for e in ENGS:
            o = Op(e, None, False)
            o.deps = [x for x in lasts if not (x.eng == e and not x.is_dma)]
            o.count = -1
            o.dval = dmas
            self.streams[e].append(o)

    def finalize(self):
        for e in ENGS:
            for o in self.streams[e]:
                for d in o.deps:
                    if not d.is_dma:
                        d.signal = True
        for e in ENGS:
            c = 0
            for o in self.streams[e]:
                if o.fn is None:
                    continue
                if o.signal and not o.is_dma:
                    c += 1
                    o.count = c
        nc = self.nc
        block = self.es.enter_context(nc.Block())

        def run(ename, engobj):
            waited = {}

            def wait(sem, val):
                if waited.get(id(sem), 0) >= val:
                    return
                waited[id(sem)] = val
                engobj.wait_ge(sem.h, val)

            for o in self.streams[ename]:
                need = {}
                for d in o.deps:
                    if d.is_dma:
                        s, v = d.dsem, d.dval
                    else:
                        s, v = self.msem[d.eng], d.count
                    if need.get(id(s), (None, 0))[1] < v:
                        need[id(s)] = (s, v)
                if o.fn is None:
                    for (s, v) in o.dval:
                        if need.get(id(s), (None, 0))[1] < v:
                            need[id(s)] = (s, v)
                for (s, v) in need.values():
                    wait(s, v)
                if o.fn is None:
                    continue
                ins = o.fn(engobj)
                if o.is_dma:
                    ins.then_inc(o.dsem.h, 16)
                elif o.signal:
                    ins.then_inc(self.msem[ename].h, 1)

        @block.tensor
        def _(e):
            run("pe", e)

        @block.scalar
        def _(e):
            run("act", e)

        @block.vector
        def _(e):
            run("dve", e)

        @block.gpsimd
        def _(e):
            run("pool", e)

        @block.sync
        def _(e):
            run("sp", e)


def host_consts():
    c = {}
    c["ident_bf"] = np.eye(128, dtype=np.float32).astype(bf16)
    c["ident_f"] = np.eye(128, dtype=np.float32)
    lt = (np.arange(128)[:, None] < np.arange(128)[None, :]).astype(np.float32)
    c["ltri"] = lt.astype(bf16)
    c["ones_bf"] = np.ones((128, 128), np.float32).astype(bf16)
    onesrow = np.zeros((128, 128), np.float32)
    onesrow[0, :] = 1.0
    c["onesrow"] = onesrow.astype(bf16)
    c["ecolm"] = np.tile((np.arange(NE, dtype=np.float32) * CAP - TRASH)[None, :], (128, 1)).astype(np.float32)
    j = np.arange(128)[:, None].astype(np.float64)
    x = np.arange(GB + GB - 128)[None, :].astype(np.float64)
    tzb = np.stack([np.exp(-SLOPES[i] * np.abs(x - j - (GB - 128))) for i in IDX_B])
    c["tzb"] = tzb.astype(np.float32).astype(bf16)
    xa = np.arange(GA + 19 * 128)[None, :].astype(np.float64)
    dl = xa - j - 1408
    ad = np.abs(dl)
    mult = (ad <= 64).astype(np.float64) + ((np.mod(dl, 4) == 0) & (ad <= 256)) + ((np.mod(dl, 16) == 0) & (ad <= 1024))
    tza = np.stack([mult * np.exp(-SLOPES[i] * ad) for i in IDX_A])
    c["tza"] = tza.astype(np.float32).astype(bf16)
    tl = np.arange(S) % GB
    hi = (tl // 32) * 32
    lo = tl % 32
    qaug = np.zeros((2, 2, S), np.float32)
    qaug[0, 0] = hi
    qaug[0, 1] = lo
    qaug[1, 0] = -hi
    qaug[1, 1] = -lo
    c["qaug"] = qaug.astype(bf16)
    kaug = np.zeros((4, 2, S), np.float32)
    for h, i in enumerate(IDX_B):
        kaug[h] = -8.0 * SLOPES[i]
    c["kaug"] = kaug.astype(bf16)
    ab = np.zeros((128, 4, 63), np.float32)
    p = np.arange(128, dtype=np.float64)
    for h, i in enumerate(IDX_B):
        for r in range(63):
            rel = r - 31
            if rel < 0:
                ab[:, h, r] = SLOPES[i] * (p + 128 * rel)
            else:
                ab[:, h, r] = -SLOPES[i] * (128 * rel + p)
    c["abias"] = ab.reshape(128, 4 * 63)
    c["mhalf"] = np.full((128, 8), -0.5, np.float32)
    return c


CONST_SPECS = None


def build(debug=None):
    debug = debug or {}
    last_phase = debug.get("last_phase", 5)
    nc = bass.Bass("TRN2", target_bir_lowering=False)
    consts = host_consts()

    def din(name, shape, dt):
        return nc.dram_tensor(name, list(shape), dt, kind="ExternalInput")

    xT = din("xT", [D, S], F32)
    xtok = din("x", [S, D], F32)
    w_in = din("w_in", [D, 5120], F32)
    w_pa = din("w_proj_a", [512, D], F32)
    w_pb = din("w_proj_b", [512, D], F32)
    w_out = din("w_out", [D, D], F32)
    w_router = din("w_router", [D, NE], F32)
    w_up = din("w_up", [NE, D, 2 * D], F32)
    w_down = din("w_down", [NE, D, D], F32)
    b_down = din("b_down", [NE, D], F32)
    bqk_d = din("bqk", [128, 16], F32)
    bv_d = din("bv_bc", [128, 1024], F32)
    bg_d = din("bg", [128, 16], F32)
    bout_d = din("bout_bc", [128, D], F32)
    ln1g_d = din("ln1g_bc", [128, D], F32)
    ln1b_d = din("ln1b_bc", [128, D], F32)
    ln2g_d = din("ln2g_bc", [128, D], F32)
    ln2b_d = din("ln2b_bc", [128, D], F32)
    subg_d = din("subg_bc", [128, 128], F32)
    lam_d = din("lam_bc", [128, 256], F32)
    brt_d = din("brouter_bc", [128, NE], F32)
    bupT_d = din("b_upT", [128, NE * 16], F32)
    cd = {}
    for k, v in consts.items():
        dt = BF16 if v.dtype == bf16 else F32
        cd[k] = din("c_" + k, v.shape, dt)
    out_d = nc.dram_tensor("out", [S, D], F32, kind="ExternalOutput")

    dbg_outs = debug.get("outs", ())

    def scr(name, shape, dt):
        if name in dbg_outs:
            return nc.dram_tensor(name, list(shape), dt, kind="ExternalOutput")
        return nc.dram_tensor(name, list(shape), dt)

    QTa = scr("QTa", [512, S], BF16)
    KTa = scr("KTa", [512, S], BF16)
    QTb = scr("QTb", [512, S], BF16)
    KTb = scr("KTb", [512, S], BF16)
    VA = scr("VA", [S, 8 * 65], BF16)
    VB = scr("VB", [S, 4 * 129], BF16)
    OT = scr("OT", [D, S], BF16)
    X1 = scr("X1", [S, D], F32)
    XG = scr("XG", [XROWS, D], BF16)
    YY = scr("YY", [XROWS, D], BF16)

    with ExitStack() as es:
        P = Prog(nc, es)

        def sbt(scope, name, shape, dt):
            t = scope.enter_context(nc.sbuf_tensor("s_" + name, list(shape), dt))
            return t, P.buf(name)

        def pst(scope, name, shape, dt):
            t = scope.enter_context(nc.psum_tensor("p_" + name, list(shape), dt))
            return t, P.buf(name)

        def dma(eng, out, in_, sbuf_buf, reads=(), writes=(), disjoint=False):
            return P.op(eng, lambda e: e.dma_start(out=out, in_=in_), reads=reads, writes=writes,
                        dma=sbuf_buf, disjoint=disjoint)

        def load(eng, t, b, src, disjoint=False):
            return dma(eng, t, src, b, writes=(b,), disjoint=disjoint)

        def store(eng, dst, t, b):
            return dma(eng, dst, t, b, reads=(b,))

        ident_bf, ident_bf_b = sbt(es, "ident_bf", [128, 128], BF16)
        ident_f, ident_f_b = sbt(es, "ident_f", [128, 128], F32)
        gates_all, gates_b = sbt(es, "gates_all", [128, NT, 4], F32)
        idx_all, idx_b = sbt(es, "idx_all", [128, NT, 4], I32)
        load("sp", ident_bf[:], ident_bf_b, cd["ident_bf"].ap())
        load("sp", ident_f[:], ident_f_b, cd["ident_f"].ap())

        def phase1():
          with ExitStack() as ph:
            wqkv, wqkv_b = sbt(ph, "wqkv", [128, 8, 3072], BF16)
            wq_h = [wqkv_b, P.buf("wqkv_h1")]
            xg = [sbt(ph, "xg%d" % i, [128, 8, 512], BF16) for i in range(2)]
            bqk, bqk_b = sbt(ph, "bqk", [128, 16], F32)
            bvbc, bvbc_b = sbt(ph, "bvbc", [128, 1024], F32)
            stg = [sbt(ph, "stg%d" % i, [128, 16, 512], BF16) for i in range(2)]
            vas = [sbt(ph, "vas%d" % i, [128, 8, 65], BF16) for i in range(4)]
            vbs = [sbt(ph, "vbs%d" % i, [128, 4, 129], BF16) for i in range(4)]
            pss = [pst(ph, "p1ps%d" % i, [128, 512], F32) for i in range(6)]
            w_in_r = w_in.ap().rearrange("(kc p) n -> p kc n", p=128)
            for hf in range(2):
                load("pool", wqkv[:, :, hf * 1536:(hf + 1) * 1536], wq_h[hf],
                     w_in_r[:, :, hf * 1536:(hf + 1) * 1536])
            load("sp", bqk[:], bqk_b, bqk_d.ap())
            load("sp", bvbc[:], bvbc_b, bv_d.ap())
            for i in range(4):
                P.op("pool", lambda e, t=vas[i][0]: e.memset(t[:, :, 64:65], 1.0), writes=(vas[i][1],))
                P.op("pool", lambda e, t=vbs[i][0]: e.memset(t[:, :, 128:129], 1.0), writes=(vbs[i][1],))
            qk_tiles = []
            for i in range(4):
                qk_tiles.append((128 * i, QTa, i))
            for i in range(4):
                qk_tiles.append((512 + 128 * i, KTa, i))
            for i in range(4):
                qk_tiles.append((1536 + 128 * i, QTb, i))
            for i in range(4):
                qk_tiles.append((2048 + 128 * i, KTb, i))
            xT_r = xT.ap().rearrange("(kc p) t -> p kc t", p=128)
            pi = 0
            for tg in range(8):
                xgt, xgb = xg[tg % 2]
                load("pool", xgt[:], xgb, xT_r[:, :, tg * 512:(tg + 1) * 512])
                st, stb = stg[tg % 2]
                for mi, (c0, dst, di) in enumerate(qk_tiles):
                    ps, psb = pss[pi % 6]
                    pi += 1
                    for kc in range(8):
                        P.op("pe", lambda e, ps=ps, kc=kc, c0=c0, xgt=xgt: e.matmul(
                            ps[:], lhsT=wqkv[:, kc, c0:c0 + 128], rhs=xgt[:, kc, :], start=(kc == 0), stop=(kc == 7)),
                            reads=(wq_h[c0 // 1536], xgb), writes=(psb,))
                    if mi % 2 == 0:
                        P.op("act", lambda e, ps=ps, st=st, mi=mi: e.activation(
                            out=st[:, mi, :], in_=ps[:], func=AF.Identity, bias=bqk[:, mi:mi + 1], scale=1.0),
                            reads=(psb, bqk_b), writes=(stb,), disjoint=True)
                    else:
                        P.op("dve", lambda e, ps=ps, st=st, mi=mi: e.tensor_scalar(
                            out=st[:, mi, :], in0=ps[:], scalar1=bqk[:, mi:mi + 1], scalar2=None, op0=ALU.add),
                            reads=(psb, bqk_b), writes=(stb,), disjoint=True)
                for gi, dst in enumerate((QTa, KTa, QTb, KTb)):
                    store("sp", dst.ap()[:, tg * 512:(tg + 1) * 512].rearrange("(m p) t -> p m t", p=128),
                          st[:, gi * 4:(gi + 1) * 4, :], stb)
                for tt in range(4):
                    T = tg * 4 + tt
                    for which in range(2):
                        ps, psb = pss[pi % 6]
                        pi += 1
                        c0 = 1024 if which == 0 else 2560
                        for kc in range(8):
                            P.op("pe", lambda e, ps=ps, kc=kc, c0=c0, xgt=xgt, tt=tt: e.matmul(
                                ps[:], lhsT=xgt[:, kc, tt * 128:(tt + 1) * 128], rhs=wqkv[:, kc, c0:c0 + 512],
                                start=(kc == 0), stop=(kc == 7)), reads=(wq_h[c0 // 1536], xgb), writes=(psb,))
                        if which == 0:
                            vt, vb_ = vas[T % 4]
                            P.op("dve", lambda e, ps=ps, vt=vt: e.tensor_tensor(
                                out=vt[:, :, 0:64], in0=ps[:].rearrange("p (h d) -> p h d", d=64),
                                in1=bvbc[:, 0:512].rearrange("p (h d) -> p h d", d=64), op=ALU.add),
                                reads=(psb, bvbc_b), writes=(vb_,), disjoint=True)
                            store("sp", VA.ap()[T * 128:(T + 1) * 128, :], vt[:].rearrange("p h c -> p (h c)"), vb_)
                        else:
                            vt, vb_ = vbs[T % 4]
                            P.op("dve", lambda e, ps=ps, vt=vt: e.tensor_tensor(
                                out=vt[:, :, 0:128], in0=ps[:].rearrange("p (h d) -> p h d", d=128),
                                in1=bvbc[:, 512:1024].rearrange("p (h d) -> p h d", d=128), op=ALU.add),
                                reads=(psb, bvbc_b), writes=(vb_,), disjoint=True)
                            store("sp", VB.ap()[T * 128:(T + 1) * 128, :], vt[:].rearrange("p h c -> p (h c)"), vb_)
            P.barrier()
            P.release([wqkv_b, wq_h[1], bqk_b, bvbc_b] + [b for _, b in xg + stg + vas + vbs])

        def phase2a():
          with ExitStack() as ph:
            Kt = [[sbt(ph, "Kt%d%d" % (s_, u), [128, S], BF16) for u in range(2)] for s_ in range(2)]
            Qb = [[sbt(ph, "Qb%d%d" % (s_, u), [128, S], BF16) for u in range(2)] for s_ in range(2)]
            Qa = [[sbt(ph, "Qa%d%d" % (s_, u), [128, S], BF16) for u in range(2)] for s_ in range(2)]
            Vh = [sbt(ph, "Vh%d" % s_, [128, NT, 129], BF16) for s_ in range(2)]
            tzb = [sbt(ph, "tzb%d" % s_, [128, 1920], BF16) for s_ in range(2)]
            abias, abias_b = sbt(ph, "abias", [128, 4 * 63], F32)
            subg, subg_b = sbt(ph, "subg", [128, 128], F32)
            lamin, lamin_b = sbt(ph, "lamin", [128, 256], F32)
            lamt, lamt_b = sbt(ph, "lamt", [128, 8], F32)
            mhalf, mhalf_b = sbt(ph, "mhalf", [128, 8], F32)
            junk, junk_b = sbt(ph, "junk", [128, 128], F32)
            PT = [sbt(ph, "PT%d" % i, [128, GB], BF16) for i in range(4)]
            o1, o1_b = sbt(ph, "o1", [128, 8, 128], F32)
            of, of_b = sbt(ph, "of", [128, 8, 128], F32)
            obf, obf_b = sbt(ph, "obf", [128, 8, 128], BF16)
            rz, rz_b = sbt(ph, "rz", [128, 32], F32)
            otst = [sbt(ph, "otst%d" % i, [128, 512], BF16) for i in range(2)]
            accsb, accsb_b = sbt(ph, "accsb", [128, 3, 480], F32)
            ST = [pst(ph, "ST%d" % i, [128, GB], F32) for i in range(2)]
            acc, acc_b = pst(ph, "acc", [128, 1536], F32)
            tp, tp_b = pst(ph, "tp2", [128, 512], BF16)

            def acc_ap(qt, c0, c1):
                base = (qt // 3) * 512 + (qt % 3) * 160
                return acc[:, base + c0:base + c1]

            load("sp", abias[:], abias_b, cd["abias"].ap())
            load("sp", subg[:], subg_b, subg_d.ap())
            load("sp", lamin[:], lamin_b, lam_d.ap())
            load("sp", mhalf[:], mhalf_b, cd["mhalf"].ap())
            for s_ in range(2):
                for u in range(2):
                    for (t, b) in (Kt[s_][u], Qb[s_][u], Qa[s_][u]):
                        P.op("pool", lambda e, t=t: e.memset(t[64:126, :], 0.0), writes=(b,))
            P.op("dve", lambda e: e.scalar_tensor_tensor(out=junk[:, 0:64], in0=lamin[:, 0:64], scalar=1.0, in1=lamin[:, 64:128],
                                                         op0=ALU.mult, op1=ALU.mult, accum_out=lamt[:, 0:1]),
                 reads=(lamin_b,), writes=(junk_b, lamt_b))
            P.op("dve", lambda e: e.scalar_tensor_tensor(out=junk[:, 64:128], in0=lamin[:, 128:192], scalar=1.0, in1=lamin[:, 192:256],
                                                         op0=ALU.mult, op1=ALU.mult, accum_out=lamt[:, 1:2]),
                 reads=(lamin_b, lamt_b), writes=(junk_b, lamt_b))
            P.op("act", lambda e: e.activation(out=lamt[:, 2:4], in_=lamt[:, 0:2], func=AF.Exp), reads=(lamt_b,), writes=(lamt_b,))
            P.op("dve", lambda e: e.tensor_tensor(out=lamt[:, 4:5], in0=lamt[:, 3:4], in1=lamt[:, 2:3], op=ALU.subtract),
                 reads=(lamt_b,), writes=(lamt_b,))
            P.op("dve", lambda e: e.tensor_scalar(out=lamt[:, 4:5], in0=lamt[:, 4:5], scalar1=-LAMBDA_INIT, scalar2=None, op0=ALU.add),
                 reads=(lamt_b,), writes=(lamt_b,))
            neglam = lamt[:, 4:5]

            def load_head(h):
                s_ = h % 2
                for u in range(2):
                    r0 = (2 * h + u) * 64
                    for (t, b), src, aug in ((Kt[s_][u], KTb, cd["kaug"].ap()[h]),
                                             (Qb[s_][u], QTb, cd["qaug"].ap()[0]),
                                             (Qa[s_][u], QTb, cd["qaug"].ap()[1])):
                        load("sp", t[0:64, :], b, src.ap()[r0:r0 + 64, :], disjoint=True)
                        load("sp", t[126:128, :], b, aug, disjoint=True)
                load("sp", Vh[s_][0][:], Vh[s_][1], VB.ap()[:, h * 129:(h + 1) * 129].rearrange("(k p) c -> p k c", p=128))
                load("sp", tzb[s_][0][:], tzb[s_][1], cd["tzb"].ap()[h])

            c_rs = 1.0 / ((1.0 - LAMBDA_INIT) ** 2)
            SUBLN_EPS = 1e-5
            steps = []
            for h in range(4):
                for g in range(4):
                    for u in range(2):
                        for kb in range(NT):
                            steps.append((h, g, u, kb))
            load_head(0)
            ot_i = [0]
            deferred = []
            cur_i = [0]

            def sacc(qt, c0, c1):
                return accsb[:, qt // 3, (qt % 3) * 160 + c0:(qt % 3) * 160 + c1]

            def finalize_unit(h, g, u):
                q0 = g * GB
                for bk in range(3):
                    w = 480 if bk < 2 else 320
                    P.op("dve", lambda e, bk=bk, w=w: e.tensor_copy(
                        out=accsb[:, bk, 0:w].rearrange("p (a c) -> p a c", c=160)[:, :, 0:129],
                        in_=acc[:, bk * 512:bk * 512 + w].rearrange("p (a c) -> p a c", c=160)[:, :, 0:129]),
                         reads=(acc_b,), writes=(accsb_b,), disjoint=(bk > 0))
                if u == 0:
                    for qt in range(8):
                        P.op("dve", lambda e, qt=qt: e.reciprocal(out=rz[:, qt:qt + 1], in_=sacc(qt, 128, 129)),
                             reads=(accsb_b,), writes=(rz_b,), disjoint=True)
                        P.op("dve", lambda e, qt=qt: e.tensor_scalar(out=o1[:, qt, :], in0=sacc(qt, 0, 128), scalar1=rz[:, qt:qt + 1],
                                                                      scalar2=None, op0=ALU.mult),
                             reads=(accsb_b, rz_b), writes=(o1_b,), disjoint=True)
                    return
                for qt in range(8):
                    P.op("dve", lambda e, qt=qt: e.reciprocal(out=rz[:, 8 + qt:9 + qt], in_=sacc(qt, 128, 129)),
                         reads=(accsb_b,), writes=(rz_b,), disjoint=True)
                    P.op("dve", lambda e, qt=qt: e.tensor_scalar(out=rz[:, 16 + qt:17 + qt], in0=rz[:, 8 + qt:9 + qt], scalar1=neglam,
                                                                  scalar2=None, op0=ALU.mult),
                         reads=(rz_b, lamt_b), writes=(rz_b,))
                    P.op("dve", lambda e, qt=qt: e.scalar_tensor_tensor(out=of[:, qt, :], in0=sacc(qt, 0, 128), scalar=rz[:, 16 + qt:17 + qt],
                                                                         in1=o1[:, qt, :], op0=ALU.mult, op1=ALU.add),
                         reads=(accsb_b, rz_b, o1_b), writes=(of_b,), disjoint=True)
                    P.op("dve", lambda e, qt=qt: e.scalar_tensor_tensor(out=junk[:], in0=of[:, qt, :], scalar=1.0, in1=of[:, qt, :],
                                                                         op0=ALU.mult, op1=ALU.mult, accum_out=rz[:, 24 + qt:25 + qt]),
                         reads=(of_b, rz_b), writes=(junk_b, rz_b))
                P.op("dve", lambda e: e.tensor_scalar(out=rz[:, 8:16], in0=rz[:, 24:32], scalar1=c_rs / 128.0, scalar2=SUBLN_EPS * c_rs,
                                                      op0=ALU.mult, op1=ALU.add), reads=(rz_b,), writes=(rz_b,))
                P.op("pool", lambda e: e.tensor_tensor(out=rz[:, 16:24], in0=rz[:, 8:16], in1=mhalf[:, 0:8], op=ALU.pow),
                     reads=(rz_b, mhalf_b), writes=(rz_b,))
                for qt in range(8):
                    P.op("dve", lambda e, qt=qt: e.scalar_tensor_tensor(out=obf[:, qt, :], in0=of[:, qt, :], scalar=rz[:, 16 + qt:17 + qt],
                                                                         in1=subg[:], op0=ALU.mult, op1=ALU.mult),
                         reads=(of_b, rz_b, subg_b), writes=(obf_b,), disjoint=True)
                def partB(h=h, q0=q0):
                    for half in range(2):
                        for j in range(4):
                            qt = half * 4 + j
                            P.op("pe", lambda e, qt=qt, j=j: e.transpose(tp[:, j * 128:(j + 1) * 128], obf[:, qt, :], ident_bf[:]),
                                 reads=(obf_b, ident_bf_b), writes=(tp_b,), disjoint=(j > 0))
                        ott, otb = otst[ot_i[0] % 2]
                        ot_i[0] += 1
                        P.op("dve", lambda e, ott=ott: e.tensor_copy(out=ott[:], in_=tp[:]), reads=(tp_b,), writes=(otb,))
                        store("sp", OT.ap()[512 + h * 128:512 + (h + 1) * 128, q0 + half * 512:q0 + (half + 1) * 512], ott[:], otb)
                if DEFER_A:
                    deferred.append([cur_i[0] + 26, partB])
                else:
                    partB()

            def emit_pv(i):
                h, g, u, kb = steps[i]
                s_ = h % 2
                ptt, ptb = PT[i % 4]
                vt, vb_ = Vh[s_]
                for qt in range(8):
                    first = (kb == 0 and qt % 3 == 0)
                    P.op("pe", lambda e, qt=qt, kb=kb, ptt=ptt, vt=vt, first=first: e.matmul(
                        acc_ap(qt, 0, 129), lhsT=ptt[:, qt * 128:(qt + 1) * 128], rhs=vt[:, kb, :],
                        start=first, stop=(kb == NT - 1), skip_group_check=True),
                        reads=(ptb, vb_), writes=(acc_b,), disjoint=(not (kb == 0 and qt == 0)))
                if kb == NT - 1:
                    finalize_unit(h, g, u)

            n = len(steps)
            for i in range(n + 2):
                if i < n:
                    h, g, u, kb = steps[i]
                    s_ = h % 2
                    q0 = g * GB
                    k0 = kb * 128
                    rel = kb - g * (GB // 128)
                    (kt_t, kt_b) = Kt[s_][u]
                    if rel < 0:
                        (q_t, q_b), kr = Qb[s_][u], 128
                    elif rel >= GB // 128:
                        (q_t, q_b), kr = Qa[s_][u], 128
                    else:
                        (q_t, q_b), kr = Qb[s_][u], 126
                    stt, stb_ = ST[i % 2]
                    for c in range(GB // 512):
                        P.op("pe", lambda e, stt=stt, kt_t=kt_t, q_t=q_t, kr=kr, k0=k0, q0=q0, c=c: e.matmul(
                            stt[:, c * 512:(c + 1) * 512], lhsT=kt_t[0:kr, k0:k0 + 128], rhs=q_t[0:kr, q0 + c * 512:q0 + (c + 1) * 512],
                            start=True, stop=True), reads=(kt_b, q_b), writes=(stb_,), disjoint=(c > 0))
                    ptt, ptb = PT[i % 4]
                    if 0 <= rel < GB // 128:
                        P.op("act", lambda e, ptt=ptt, stt=stt: e.activation(out=ptt[:], in_=stt[:], func=AF.Exp, scale=0.125),
                             reads=(stb_,), writes=(ptb,))
                        woff = (GB - 128) - 128 * rel
                        tzt, tzb_ = tzb[s_]
                        P.op("dve", lambda e, ptt=ptt, tzt=tzt, woff=woff: e.tensor_tensor(
                            out=ptt[:], in0=ptt[:], in1=tzt[:, woff:woff + GB], op=ALU.mult),
                            reads=(ptb, tzb_), writes=(ptb,))
                    else:
                        col = h * 63 + rel + 31
                        P.op("act", lambda e, ptt=ptt, stt=stt, col=col: e.activation(
                            out=ptt[:], in_=stt[:], func=AF.Exp, bias=abias[:, col:col + 1], scale=0.125),
                            reads=(stb_, abias_b), writes=(ptb,))
                cur_i[0] = i
                if i >= 2:
                    emit_pv(i - 2)
                while deferred and deferred[0][0] <= i:
                    deferred.pop(0)[1]()
                if 1 <= i <= n:
                    h, g, u, kb = steps[i - 1]
                    if g == 0 and u == 0 and kb == 0 and h + 1 < 4:
                        load_head(h + 1)
            while deferred:
                deferred.pop(0)[1]()
            P.barrier()
            P.release([b for pair in Kt + Qb + Qa for (_, b) in pair] + [b for _, b in Vh + tzb + PT + otst] +
                      [abias_b, subg_b, lamin_b, mhalf_b])

        def phase2b():
          with ExitStack() as ph:
            Qh = [sbt(ph, "Qh%d" % s_, [128, S], BF16) for s_ in range(2)]
            Kh = [sbt(ph, "Kh%d" % s_, [128, S], BF16) for s_ in range(2)]
            tza = [sbt(ph, "tza%d" % s_, [128, 2944], BF16) for s_ in range(2)]
            Va, Va_b = sbt(ph, "Va", [128, NT, 520], BF16)
            PT = [sbt(ph, "PTa%d" % i, [128, GA], BF16) for i in range(5)]
            oabs = [sbt(ph, "oab%d" % i, [128, 4, 128], BF16) for i in range(2)]
            rz, rz_b = sbt(ph, "rza", [128, 4], F32)
            otst = [sbt(ph, "otsa%d" % i, [64, 512], BF16) for i in range(2)]
            ST = [pst(ph, "STa%d" % i, [128, GA], F32) for i in range(4)]
            accs = [pst(ph, "acca%d" % i, [128, 512], F32) for i in range(2)]
            tp, tp_b = pst(ph, "tp2a", [128, 512], BF16)
            load("sp", Va[:], Va_b, VA.ap().rearrange("(k p) c -> p k c", p=128))
            for (t_, b_) in oabs:
                P.op("pool", lambda e, t_=t_: e.memset(t_[:], 0.0), writes=(b_,))
            for s_ in range(2):
                P.op("pool", lambda e, t=Qh[s_][0]: e.memset(t[64:128, :], 0.0), writes=(Qh[s_][1],))
                P.op("pool", lambda e, t=Kh[s_][0]: e.memset(t[64:128, :], 0.0), writes=(Kh[s_][1],))

            def load_head_a(h):
                s_ = h % 2
                load("sp", Qh[s_][0][0:64, :], Qh[s_][1], QTa.ap()[h * 64:(h + 1) * 64, :], disjoint=True)
                load("sp", Kh[s_][0][0:64, :], Kh[s_][1], KTa.ap()[h * 64:(h + 1) * 64, :], disjoint=True)
                load("sp", tza[s_][0][:], tza[s_][1], cd["tza"].ap()[h])

            steps = []
            for h in range(8):
                for g in range(8):
                    kbs = [kb for kb in range(4 * g - 8, 4 * g + 12) if 0 <= kb < NT]
                    for j, kb in enumerate(kbs):
                        steps.append((h, g, kb, j == 0, j == len(kbs) - 1))
            load_head_a(0)
            ot_i = [0]
            unit_i = [0]
            deferred = []
            cur_i = [0]

            def acc_of(ui, qt, c0, c1):
                a, ab_ = accs[ui % 2]
                return a[:, qt * 80 + c0:qt * 80 + c1], ab_

            def finalize_a(h, g, ui):
                q0 = g * GA
                oab, oab_b = oabs[ui % 2]
                for qt in range(4):
                    zap, ab_ = acc_of(ui, qt, 64, 65)
                    uap, _ = acc_of(ui, qt, 0, 64)
                    P.op("dve", lambda e, qt=qt, zap=zap: e.reciprocal(out=rz[:, qt:qt + 1], in_=zap),
                         reads=(ab_,), writes=(rz_b,), disjoint=True)
                    P.op("dve", lambda e, qt=qt, uap=uap: e.tensor_scalar(out=oab[:, qt, 0:64], in0=uap, scalar1=rz[:, qt:qt + 1],
                                                                          scalar2=None, op0=ALU.mult),
                         reads=(ab_, rz_b), writes=(oab_b,), disjoint=True)
                def partB(h=h, q0=q0, oab=oab, oab_b=oab_b):
                    for qt in range(4):
                        P.op("pe", lambda e, qt=qt: e.transpose(tp[:, qt * 128:(qt + 1) * 128], oab[:, qt, :], ident_bf[:]),
                             reads=(oab_b, ident_bf_b), writes=(tp_b,), disjoint=(qt > 0))
                    ott, otb = otst[ot_i[0] % 2]
                    ot_i[0] += 1
                    P.op("dve", lambda e, ott=ott: e.tensor_copy(out=ott[:], in_=tp[0:64, :]), reads=(tp_b,), writes=(otb,))
                    store("sp", OT.ap()[h * 64:(h + 1) * 64, q0:q0 + GA], ott[:], otb)
                if DEFER_B:
                    deferred.append([cur_i[0] + 9, partB])
                else:
                    partB()

            def emit_pv_a(i, ui):
                h, g, kb, first_kb, last_kb = steps[i]
                ptt, ptb = PT[i % 5]
                for qt in range(4):
                    oap, ab_ = acc_of(ui, qt, 0, 65)
                    P.op("pe", lambda e, qt=qt, kb=kb, ptt=ptt, oap=oap, first=(first_kb and qt == 0), h=h, last_kb=last_kb: e.matmul(
                        oap, lhsT=ptt[:, qt * 128:(qt + 1) * 128], rhs=Va[:, kb, h * 65:(h + 1) * 65],
                        start=first, stop=last_kb, skip_group_check=True),
                        reads=(ptb, Va_b), writes=(ab_,), disjoint=(not (first_kb and qt == 0)))
                if last_kb:
                    finalize_a(h, g, ui)

            n = len(steps)
            uis = []
            ui = -1
            for i in range(n):
                if steps[i][3]:
                    ui += 1
                uis.append(ui)
            for i in range(n + 3):
                if i < n:
                    h, g, kb, first_kb, last_kb = steps[i]
                    s_ = h % 2
                    if g == 0 and first_kb and h + 1 < 8:
                        load_head_a(h + 1)
                    q0 = g * GA
                    k0 = kb * 128
                    stt, stb_ = ST[i % 4]
                    P.op("pe", lambda e, stt=stt, kt=Kh[s_][0], qt_=Qh[s_][0], k0=k0, q0=q0: e.matmul(
                        stt[:], lhsT=kt[:, k0:k0 + 128], rhs=qt_[:, q0:q0 + GA], start=True, stop=True),
                        reads=(Kh[s_][1], Qh[s_][1]), writes=(stb_,))
                    ptt, ptb = PT[i % 5]
                    P.op("act", lambda e, ptt=ptt, stt=stt: e.activation(out=ptt[:], in_=stt[:], func=AF.Exp, scale=0.125),
                         reads=(stb_,), writes=(ptb,))
                    b_ = kb - 4 * g + 8
                    woff = 2432 - 128 * b_
                    tzt, tzb_ = tza[s_]
                    meng = "dve"
                    P.op(meng, lambda e, ptt=ptt, tzt=tzt, woff=woff: e.tensor_tensor(
                        out=ptt[:], in0=ptt[:], in1=tzt[:, woff:woff + GA], op=ALU.mult),
                        reads=(ptb, tzb_), writes=(ptb,))
                cur_i[0] = i
                if i >= 3:
                    emit_pv_a(i - 3, uis[i - 3])
                while deferred and deferred[0][0] <= i:
                    deferred.pop(0)[1]()
            while deferred:
                deferred.pop(0)[1]()
            P.barrier()
            P.release([b for _, b in Qh + Kh + tza + PT + otst + oabs] + [Va_b])

        def phase3():
          with ExitStack() as ph:
            wg, wg_b = pre3["wg"]
            wpa, wpa_b = pre3["wpa"]
            wpb, wpb_b = pre3["wpb"]
            wout, wout_b = pre3["wout"]
            wr, wr_b = sbt(ph, "wr", [128, 8, NE], F32)
            bg, bg_b = sbt(ph, "bg", [128, 16], F32)
            boutb, boutb_b = sbt(ph, "boutb", [128, D], F32)
            l1g, l1g_b = sbt(ph, "l1g", [128, D], F32)
            l1b, l1b_b = sbt(ph, "l1b", [128, D], F32)
            brt, brt_b = sbt(ph, "brt", [128, NE], F32)
            ltri, ltri_b = sbt(ph, "ltri", [128, 128], BF16)
            onesb, onesb_b = sbt(ph, "onesb", [128, 128], BF16)
            ecolm, ecolm_b = sbt(ph, "ecolm", [128, NE], F32)
            mhalf, mhalf_b = sbt(ph, "mhalf3", [128, 8], F32)
            cum, cum_b = sbt(ph, "cum", [128, NE], BF16)
            onesrow3, onesrow3_b = sbt(ph, "onesrow3", [128, 128], BF16)
            boutp, boutp_b = sbt(ph, "boutp", [128, D], BF16)
            xg = [sbt(ph, "xg3_%d" % i, [128, 8, 512], BF16) for i in range(2)]
            otg = [sbt(ph, "otg%d" % i, [128, 8, 512], BF16) for i in range(2)]
            mT = [sbt(ph, "mT%d" % i, [128, 8, 512], BF16) for i in range(2)]
            sa = [sbt(ph, "sa%d" % i, [128, 512], F32) for i in range(2)]
            sb_ = [sbt(ph, "sb%d" % i, [128, 512], F32) for i in range(2)]
            t1 = [sbt(ph, "t1_%d" % i, [128, 512], F32) for i in range(2)]
            t2 = [sbt(ph, "t2_%d" % i, [128, 512], F32) for i in range(2)]
            xt_ = [sbt(ph, "xtk%d" % i, [128, D], F32) for i in range(2)]
            zt = [sbt(ph, "zt%d" % i, [128, D], F32) for i in range(2)]
            x1t = [sbt(ph, "x1t%d" % i, [128, D], F32) for i in range(4)]
            x1bf = [sbt(ph, "x1bf%d" % i, [128, D], BF16) for i in range(4)]
            x1T = [sbt(ph, "x1T%d" % i, [128, 8, 128], F32) for i in range(2)]
            sm = [sbt(ph, "sm%d" % i, [128, 400], F32) for i in range(3)]
            lns = [sbt(ph, "lns%d" % i, [128, 32], F32) for i in range(2)]
            mbf = [sbt(ph, "mbf%d" % i, [128, NE], BF16) for i in range(3)]
            pool4 = [pst(ph, "p3ps%d" % i, [128, 512], F32) for i in range(4)]
            yps = [pst(ph, "yps", [128, 1024], F32)]
            xTp = [pst(ph, "xTp", [128, 512], F32)]
            lgp = [pst(ph, "lgp", [128, 64], F32)]
            pos_b = P.buf("pos_b")
            w_in_r = w_in.ap().rearrange("(kc p) n -> p kc n", p=128)
            load("sp", wr[:], wr_b, w_router.ap().rearrange("(kc p) n -> p kc n", p=128))
            for (t, b, src) in ((bg, bg_b, bg_d), (boutb, boutb_b, bout_d), (l1g, l1g_b, ln1g_d), (l1b, l1b_b, ln1b_d),
                                (brt, brt_b, brt_d), (ltri, ltri_b, cd["ltri"]), (onesb, onesb_b, cd["ones_bf"]),
                                (ecolm, ecolm_b, cd["ecolm"]), (mhalf, mhalf_b, cd["mhalf"])):
                load("sp", t[:], b, src.ap())
            P.op("pool", lambda e: e.memset(cum[:], 0.0), writes=(cum_b,))
            P.op("pool", lambda e: e.memset(boutp[:], 0.0), writes=(boutp_b,))
            load("pool", boutp[0:1, :], boutp_b, bout_d.ap()[0:1, :])
            load("sp", onesrow3[:], onesrow3_b, cd["onesrow"].ap())
            xT_r = xT.ap().rearrange("(kc p) t -> p kc t", p=128)
            OT_r = OT.ap().rearrange("(kc p) t -> p kc t", p=128)
            pi = [0]

            def nextps():
                r = pool4[pi[0] % 4]
                pi[0] += 1
                return r

            yp, ypb = yps[0]
            xp, xpb = xTp[0]
            lp, lpb = lgp[0]

            def S0(T, mTt, mTb, tt):
                for hf in range(2):
                    P.op("pe", lambda e, hf=hf: e.matmul(yp[:, hf * 512:(hf + 1) * 512], lhsT=onesrow3[:], rhs=boutp[:, hf * 512:(hf + 1) * 512],
                                                       start=True, stop=False), reads=(onesrow3_b, boutp_b), writes=(ypb,), disjoint=(hf > 0))
                    for m in range(8):
                        P.op("pe", lambda e, m=m, hf=hf, tt=tt, mTt=mTt: e.matmul(
                            yp[:, hf * 512:(hf + 1) * 512], lhsT=mTt[:, m, tt * 128:(tt + 1) * 128], rhs=wout[:, m, hf * 512:(hf + 1) * 512],
                            start=False, stop=(m == 7)), reads=(mTb, wout_b), writes=(ypb,), disjoint=True)

            def S1(T):
                xk, xkb = xt_[T % 2]
                z, zb = zt[T % 2]
                x1, x1b = x1t[T % 4]
                xb16, xb16b = x1bf[T % 4]
                lt, ltb = lns[T % 2]
                load("sp", xk[:], xkb, xtok.ap()[T * 128:(T + 1) * 128, :])
                P.op("dve", lambda e: e.scalar_tensor_tensor(out=z[:], in0=xk[:], scalar=ALPHA, in1=yp[:], op0=ALU.mult, op1=ALU.add),
                     reads=(xkb, ypb), writes=(zb,))
                emit_ln(P, z, zb, x1, x1b, lt, ltb, l1g, l1g_b, l1b, l1b_b, mhalf, mhalf_b, o=0, use_act=True, aff_eng=("dve", "dve"))
                store("sp", X1.ap()[T * 128:(T + 1) * 128, :], x1[:], x1b)

            def S2(T):
                x1, x1b = x1t[T % 4]
                xTt, xTb = x1T[T % 2]
                for half in range(2):
                    for j in range(4):
                        kc = half * 4 + j
                        P.op("pe", lambda e, j=j, kc=kc: e.transpose(xp[:, j * 128:(j + 1) * 128], x1[:, kc * 128:(kc + 1) * 128], ident_f[:]),
                             reads=(x1b, ident_f_b), writes=(xpb,), disjoint=(j > 0))
                    P.op("act", lambda e, half=half: e.activation(
                        out=xTt[:, half * 4:(half + 1) * 4, :], in_=xp[:].rearrange("p (a b) -> p a b", b=128), func=AF.Identity),
                        reads=(xpb,), writes=(xTb,), disjoint=(half > 0))
                for kc in range(8):
                    P.op("pe", lambda e, kc=kc: e.matmul(lp[:, 0:NE], lhsT=xTt[:, kc, :], rhs=wr[:, kc, :], start=(kc == 0), stop=(kc == 7)),
                         reads=(xTb, wr_b), writes=(lpb,), disjoint=(kc > 0))

            def S3(T):
                smt, smb = sm[T % 3]
                mb, mbb = mbf[T % 3]
                x1, x1b = x1t[T % 4]
                xb16, xb16b = x1bf[T % 4]
                P.op("act", lambda e: e.activation(out=xb16[:], in_=x1[:], func=AF.Identity), reads=(x1b,), writes=(xb16b,))
                P.op("dve", lambda e: e.tensor_tensor(out=smt[:, 0:32], in0=lp[:, 0:NE], in1=brt[:], op=ALU.add),
                     reads=(lpb, brt_b), writes=(smb,))
                P.op("dve", lambda e: e.max(out=smt[:, 32:40], in_=smt[:, 0:32]), reads=(smb,), writes=(smb,))
                P.op("dve", lambda e: e.tensor_scalar(out=smt[:, 64:96], in0=smt[:, 0:32], scalar1=smt[:, 35:36], scalar2=None, op0=ALU.is_ge),
                     reads=(smb,), writes=(smb,))
                P.op("dve", lambda e: e.tensor_scalar(out=smt[:, 40:41], in0=smt[:, 32:33], scalar1=-1.0, scalar2=None, op0=ALU.mult),
                     reads=(smb,), writes=(smb,))
                P.op("act", lambda e: e.activation(out=smt[:, 96:128], in_=smt[:, 0:32], func=AF.Sigmoid, bias=smt[:, 40:41], scale=1.0),
                     reads=(smb,), writes=(smb,))
                P.op("act", lambda e: e.activation(out=smt[:, 128:160], in_=smt[:, 0:32], func=AF.Sigmoid, bias=smt[:, 32:33], scale=-1.0),
                     reads=(smb,), writes=(smb,))
                P.op("dve", lambda e: e.reciprocal(out=smt[:, 128:160], in_=smt[:, 128:160]), reads=(smb,), writes=(smb,))
                P.op("dve", lambda e: e.tensor_tensor(out=smt[:, 160:192], in0=smt[:, 96:128], in1=smt[:, 128:160], op=ALU.mult),
                     reads=(smb,), writes=(smb,))
                P.op("dve", lambda e: e.scalar_tensor_tensor(out=smt[:, 160:192], in0=smt[:, 160:192], scalar=1.0, in1=smt[:, 64:96],
                                                             op0=ALU.mult, op1=ALU.mult, accum_out=smt[:, 192:193]),
                     reads=(smb,), writes=(smb,))
                P.op("dve", lambda e: e.reciprocal(out=smt[:, 193:194], in_=smt[:, 192:193]), reads=(smb,), writes=(smb,))
                P.op("dve", lambda e: e.tensor_scalar(out=smt[:, 224:256], in0=smt[:, 160:192], scalar1=smt[:, 193:194], scalar2=None, op0=ALU.mult),
                     reads=(smb,), writes=(smb,))
                P.op("dve", lambda e: e.tensor_copy(out=mb[:], in_=smt[:, 64:96]), reads=(smb,), writes=(mbb,))

            def S4(T):
                mb, mbb = mbf[T % 3]
                P.op("pe", lambda e: e.matmul(lp[:, 32:64], lhsT=ltri[:], rhs=mb[:], start=True, stop=False, skip_group_check=True),
                     reads=(mbb, ltri_b), writes=(pos_b,))
                P.op("pe", lambda e: e.matmul(lp[:, 32:64], lhsT=onesb[:], rhs=cum[:], start=False, stop=True, skip_group_check=True),
                     reads=(cum_b, onesb_b), writes=(pos_b,), disjoint=True)

            def S5(T):
                smt, smb = sm[T % 3]
                mb, mbb = mbf[T % 3]
                xb16, xb16b = x1bf[T % 4]
                P.op("dve", lambda e: e.tensor_scalar(out=smt[:, 288:320], in0=lp[:, 32:64], scalar1=float(CAP), scalar2=None, op0=ALU.is_lt),
                     reads=(pos_b,), writes=(smb,))
                P.op("dve", lambda e: e.tensor_tensor(out=smt[:, 256:288], in0=lp[:, 32:64], in1=ecolm[:], op=ALU.add),
                     reads=(pos_b, ecolm_b, smb), writes=(smb,))
                P.op("pool", lambda e: e.tensor_tensor(out=cum[:], in0=cum[:], in1=mb[:], op=ALU.add), reads=(cum_b, mbb), writes=(cum_b,))
                P.op("dve", lambda e: e.tensor_tensor(out=smt[:, 256:288], in0=smt[:, 256:288], in1=smt[:, 288:320], op=ALU.mult),
                     reads=(smb,), writes=(smb,))
                for kk in range(4):
                    P.op("dve", lambda e, kk=kk: e.tensor_scalar(out=smt[:, 320:352], in0=smt[:, 0:32], scalar1=smt[:, 32 + kk:33 + kk],
                                                                 scalar2=None, op0=ALU.is_equal), reads=(smb,), writes=(smb,))
                    P.op("dve", lambda e, kk=kk: e.scalar_tensor_tensor(out=smt[:, 352:384], in0=smt[:, 320:352], scalar=1.0, in1=smt[:, 256:288],
                                                                        op0=ALU.mult, op1=ALU.mult, accum_out=smt[:, 384 + kk:385 + kk]),
                         reads=(smb,), writes=(smb,))
                    P.op("dve", lambda e, kk=kk: e.scalar_tensor_tensor(out=smt[:, 352:384], in0=smt[:, 320:352], scalar=1.0, in1=smt[:, 224:256],
                                                                        op0=ALU.mult, op1=ALU.mult, accum_out=gates_all[:, T, kk:kk + 1]),
                         reads=(smb,), writes=(smb,))
                P.op("dve", lambda e: e.tensor_scalar(out=idx_all[:, T, :], in0=smt[:, 384:388], scalar1=float(TRASH), scalar2=None, op0=ALU.add),
                     reads=(smb,), writes=(idx_b,), nowar=True)
                for kk in range(4):
                    P.op("pool", lambda e, kk=kk: e.indirect_dma_start(
                        out=XG.ap()[:, :], out_offset=bass.IndirectOffsetOnAxis(ap=idx_all[:, T, kk:kk + 1], axis=0),
                        in_=xb16[:, :], in_offset=None), reads=(xb16b, idx_b), dma=xb16b)

            def load_tg(tg):
                load("pool", xg[tg % 2][0][:], xg[tg % 2][1], xT_r[:, :, tg * 512:(tg + 1) * 512])
                load("sp", otg[tg % 2][0][:], otg[tg % 2][1], OT_r[:, :, tg * 512:(tg + 1) * 512])

            for tg in range(8):
                xgt, xgb = xg[tg % 2]
                ogt, ogb = otg[tg % 2]
                mTt, mTb = mT[tg % 2]
                load_tg(tg)
                for m in range(8):
                    k = (tg * 8 + m) % 2
                    ps, psb = nextps()
                    for kc in range(8):
                        P.op("pe", lambda e, ps=ps, kc=kc, m=m, xgt=xgt: e.matmul(ps[:], lhsT=wg[:, kc, m * 128:(m + 1) * 128], rhs=xgt[:, kc, :],
                                                                                 start=(kc == 0), stop=(kc == 7)),
                             reads=(wg_b, xgb), writes=(psb,))
                    P.op("act", lambda e, ps=ps, k=k, m=m: e.activation(out=sa[k][0][:], in_=ps[:], func=AF.Sigmoid, bias=bg[:, m:m + 1], scale=1.0),
                         reads=(psb, bg_b), writes=(sa[k][1],))
                    ps, psb = nextps()
                    for kc in range(4):
                        P.op("pe", lambda e, ps=ps, kc=kc, m=m, ogt=ogt: e.matmul(ps[:], lhsT=wpa[:, kc, m * 128:(m + 1) * 128], rhs=ogt[:, kc, :],
                                                                                 start=(kc == 0), stop=(kc == 3)),
                             reads=(wpa_b, ogb), writes=(psb,))
                    P.op("dve", lambda e, ps=ps, k=k: e.tensor_tensor(out=t1[k][0][:], in0=sa[k][0][:], in1=ps[:], op=ALU.mult),
                         reads=(psb, sa[k][1]), writes=(t1[k][1],))
                    ps, psb = nextps()
                    for kc in range(8):
                        P.op("pe", lambda e, ps=ps, kc=kc, m=m, xgt=xgt: e.matmul(ps[:], lhsT=wg[:, kc, 1024 + m * 128:1024 + (m + 1) * 128],
                                                                                 rhs=xgt[:, kc, :], start=(kc == 0), stop=(kc == 7)),
                             reads=(wg_b, xgb), writes=(psb,))
                    P.op("act", lambda e, ps=ps, k=k, m=m: e.activation(out=sb_[k][0][:], in_=ps[:], func=AF.Sigmoid, bias=bg[:, 8 + m:9 + m], scale=1.0),
                         reads=(psb, bg_b), writes=(sb_[k][1],))
                    ps, psb = nextps()
                    for kc in range(4):
                        P.op("pe", lambda e, ps=ps, kc=kc, m=m, ogt=ogt: e.matmul(ps[:], lhsT=wpb[:, kc, m * 128:(m + 1) * 128], rhs=ogt[:, 4 + kc, :],
                                                                                 start=(kc == 0), stop=(kc == 3)),
                             reads=(wpb_b, ogb), writes=(psb,))
                    P.op("dve", lambda e, ps=ps, k=k: e.tensor_tensor(out=t2[k][0][:], in0=sb_[k][0][:], in1=ps[:], op=ALU.mult),
                         reads=(psb, sb_[k][1]), writes=(t2[k][1],))
                    P.op("pool", lambda e, k=k, m=m, mTt=mTt: e.tensor_tensor(out=mTt[:, m, :], in0=t1[k][0][:], in1=t2[k][0][:], op=ALU.add),
                         reads=(t1[k][1], t2[k][1]), writes=(mTb,), disjoint=True)
                for tt in range(4):
                    T = tg * 4 + tt
                    S0(T, mTt, mTb, tt)
                    if T - 2 >= 0:
                        S2(T - 2)
                    if T - 3 >= 0:
                        S4(T - 3)
                    S1(T)
                    if T - 2 >= 0:
                        S3(T - 2)
                    if T - 3 >= 0:
                        S5(T - 3)
            for T in range(NT, NT + 3):
                if T - 2 < NT:
                    S2(T - 2)
                if T - 3 < NT:
                    S4(T - 3)
                if T - 2 < NT:
                    S3(T - 2)
                if T - 3 < NT:
                    S5(T - 3)
            P.barrier()
            rel = [onesrow3_b, boutp_b, wg_b, wpa_b, wpb_b, wout_b, wr_b, bg_b, boutb_b, l1g_b, l1b_b, brt_b, ltri_b, onesb_b, ecolm_b, mhalf_b]
            rel += [b for _, b in xg + otg + xt_ + x1t + x1bf]
            P.release(rel)

        def phase4():
          with ExitStack() as ph:
            wup = [sbt(ph, "wup%d" % i, [128, 8, 2048], BF16) for i in range(2)]
            wdn = [sbt(ph, "wdn%d" % i, [128, 8, 1024], BF16) for i in range(2)]
            bdp = [sbt(ph, "bdp%d" % i, [128, 1024], BF16) for i in range(2)]
            bup, bup_b = sbt(ph, "bup", [128, NE * 16], F32)
            onesrow, onesrow_b = sbt(ph, "onesrow", [128, 128], BF16)
            zero_t, zero_b = sbt(ph, "zero_t", [128, D], BF16)
            xgr = [sbt(ph, "xgr%d" % i, [128, 5, D], BF16) for i in range(2)]
            xgT = [sbt(ph, "xgT%d" % i, [128, 8, CAP], BF16) for i in range(2)]
            actT = [sbt(ph, "actT%d" % i, [128, 8, CAP], BF16) for i in range(2)]
            yst = [sbt(ph, "yst%d" % i, [128, 5, D], BF16) for i in range(2)]
            gp = [sbt(ph, "gp%d" % i, [128, 320], F32) for i in range(2)]
            sg = [sbt(ph, "sg%d" % i, [128, 320], F32) for i in range(2)]
            tu = [sbt(ph, "tu%d" % i, [128, 320], F32) for i in range(2)]
            gs = [sbt(ph, "gs%d" % i, [128, 320], F32) for i in range(2)]
            pg = [pst(ph, "pg%d" % i, [128, 512], F32) for i in range(2)]
            pu = [pst(ph, "pu%d" % i, [128, 512], F32) for i in range(2)]
            yph = [pst(ph, "yp4_%d" % i, [128, 512], F32) for i in range(2)]
            tps_ = [pst(ph, "tp4_%d" % i, [128, 512], BF16) for i in range(2)]
            tpi = [0]
            load("sp", bup[:], bup_b, bupT_d.ap())
            load("sp", onesrow[:], onesrow_b, cd["onesrow"].ap())
            P.op("dve", lambda e: e.tensor_scalar(out=bup[:].rearrange("p (e m) -> p e m", m=16)[:, :, 8:16],
                                                  in0=bup[:].rearrange("p (e m) -> p e m", m=16)[:, :, 8:16], scalar1=1.0, scalar2=None, op0=ALU.add),
                 reads=(bup_b,), writes=(bup_b,))
            P.op("pool", lambda e: e.memset(zero_t[:], 0.0), writes=(zero_b,))
            store("sp", YY.ap()[TRASH:TRASH + 128, :], zero_t[:], zero_b)
            for i in range(2):
                P.op("pool", lambda e, t=bdp[i][0]: e.memset(t[:], 0.0), writes=(bdp[i][1],))

            def load_w(e_):
                s_ = e_ % 2
                load("pool", wup[s_][0][:], wup[s_][1], w_up.ap()[e_].rearrange("(kc p) n -> p kc n", p=128))
                load("pool", wdn[s_][0][:], wdn[s_][1], w_down.ap()[e_].rearrange("(kc p) n -> p kc n", p=128))
                load("pool", bdp[s_][0][0:1, :], bdp[s_][1], b_down.ap()[e_:e_ + 1, :])
                for kc in range(8):
                    P.op("sp", lambda e, kc=kc, e_=e_, t_=xgT[s_][0]: e.dma_start_transpose(
                        out=t_[:, kc, :], in_=XG.ap()[e_ * CAP:(e_ + 1) * CAP, kc * 128:(kc + 1) * 128]),
                        writes=(xgT[s_][1],), dma=xgT[s_][1], disjoint=(kc > 0))

            def transposes(e_):
                return
                s_ = e_ % 2
                xr, xrb = xgr[s_]
                xT_, xTb = xgT[s_]
                for t in range(5):
                    for half in range(2):
                        tp, tp_b = tps_[tpi[0] % 2]
                        tpi[0] += 1
                        for j in range(4):
                            kc = half * 4 + j
                            P.op("pe", lambda e, j=j, kc=kc, t=t, xr=xr, tp=tp: e.transpose(tp[:, j * 128:(j + 1) * 128], xr[:, t, kc * 128:(kc + 1) * 128], ident_bf[:]),
                                 reads=(xrb, ident_bf_b), writes=(tp_b,), disjoint=(j > 0))
                        P.op("act", lambda e, half=half, t=t, xT_=xT_, tp=tp: e.activation(
                            out=xT_[:, half * 4:(half + 1) * 4, t * 128:(t + 1) * 128], in_=tp[:].rearrange("p (a b) -> p a b", b=128), func=AF.Identity),
                            reads=(tp_b,), writes=(xTb,), disjoint=True)

            def up(e_):
                s_ = e_ % 2
                wu, wub = wup[s_]
                xT_, xTb = xgT[s_]
                aT, aTb = actT[s_]
                it = 0
                for m in range(8):
                    for hf in range(2):
                        n0 = hf * 320
                        k = it % 2
                        it += 1
                        pgt, pgb = pg[k]
                        put, pub = pu[k]
                        for kc in range(8):
                            P.op("pe", lambda e, kc=kc, m=m, n0=n0, pgt=pgt, wu=wu, xT_=xT_: e.matmul(
                                pgt[:, 0:320], lhsT=wu[:, kc, m * 128:(m + 1) * 128], rhs=xT_[:, kc, n0:n0 + 320], start=(kc == 0), stop=(kc == 7)),
                                reads=(wub, xTb), writes=(pgb,))
                        for kc in range(8):
                            P.op("pe", lambda e, kc=kc, m=m, n0=n0, put=put, wu=wu, xT_=xT_: e.matmul(
                                put[:, 0:320], lhsT=wu[:, kc, 1024 + m * 128:1024 + (m + 1) * 128], rhs=xT_[:, kc, n0:n0 + 320], start=(kc == 0), stop=(kc == 7)),
                                reads=(wub, xTb), writes=(pub,))
                        cg = e_ * 16 + m
                        cu = e_ * 16 + 8 + m
                        P.op("dve", lambda e, k=k, pgt=pgt, cg=cg: e.tensor_scalar(out=gp[k][0][:], in0=pgt[:, 0:320], scalar1=bup[:, cg:cg + 1], scalar2=7.0,
                                                                                  op0=ALU.add, op1=ALU.min), reads=(pgb, bup_b), writes=(gp[k][1],))
                        P.op("act", lambda e, k=k: e.activation(out=sg[k][0][:], in_=gp[k][0][:], func=AF.Sigmoid, scale=1.702),
                             reads=(gp[k][1],), writes=(sg[k][1],))
                        P.op("dve", lambda e, k=k, put=put, cu=cu: e.tensor_scalar(out=tu[k][0][:], in0=put[:, 0:320], scalar1=bup[:, cu:cu + 1], scalar2=8.0,
                                                                                  op0=ALU.add, op1=ALU.min), reads=(pub, bup_b), writes=(tu[k][1],))
                        P.op("dve", lambda e, k=k: e.tensor_tensor(out=gs[k][0][:], in0=gp[k][0][:], in1=sg[k][0][:], op=ALU.mult),
                             reads=(gp[k][1], sg[k][1]), writes=(gs[k][1],))
                        P.op("dve", lambda e, k=k, m=m, n0=n0, aT=aT: e.scalar_tensor_tensor(out=aT[:, m, n0:n0 + 320], in0=tu[k][0][:], scalar=-6.0, in1=gs[k][0][:],
                                                                                            op0=ALU.max, op1=ALU.mult),
                             reads=(tu[k][1], gs[k][1]), writes=(aTb,), disjoint=True)

            def down(e_):
                s_ = e_ % 2
                wd, wdb = wdn[s_]
                aT, aTb = actT[s_]
                bd, bdb = bdp[s_]
                ys, ysb = yst[s_]
                for t in range(5):
                    for hf in range(2):
                        yp, ypb = yph[hf]
                        P.op("pe", lambda e, hf=hf, bd=bd, yp=yp: e.matmul(yp[:], lhsT=onesrow[:], rhs=bd[:, hf * 512:(hf + 1) * 512],
                                                                    start=True, stop=False), reads=(onesrow_b, bdb), writes=(ypb,))
                        for kc in range(8):
                            P.op("pe", lambda e, hf=hf, kc=kc, t=t, aT=aT, wd=wd, yp=yp: e.matmul(
                                yp[:], lhsT=aT[:, kc, t * 128:(t + 1) * 128], rhs=wd[:, kc, hf * 512:(hf + 1) * 512],
                                start=False, stop=(kc == 7)), reads=(aTb, wdb), writes=(ypb,), disjoint=True)
                        P.op("act", lambda e, t=t, ys=ys, hf=hf, yp=yp: e.activation(out=ys[:, t, hf * 512:(hf + 1) * 512], in_=yp[:], func=AF.Identity),
                             reads=(ypb,), writes=(ysb,), disjoint=True)
                store("sp", YY.ap()[e_ * CAP:(e_ + 1) * CAP, :].rearrange("(t p) d -> p t d", p=128), ys[:], ysb)

            load_w(0)
            load_w(1)
            transposes(0)
            for e_ in range(NE):
                up(e_)
                if e_ + 1 < NE:
                    transposes(e_ + 1)
                down(e_)
                if e_ + 2 < NE:
                    load_w(e_ + 2)
            P.barrier()
            P.release([b for _, b in wup + wdn + bdp + xgr + yst] + [bup_b, onesrow_b, zero_b])

        def phase5():
          with ExitStack() as ph:
            l2g, l2g_b = sbt(ph, "l2g", [128, D], F32)
            l2b, l2b_b = sbt(ph, "l2b", [128, D], F32)
            mhalf, mhalf_b = sbt(ph, "mhalf5", [128, 8], F32)
            yk = [sbt(ph, "yk%d" % i, [128, 4, D], BF16) for i in range(3)]
            x1l = [sbt(ph, "x1l%d" % i, [128, D], F32) for i in range(3)]
            ac = [sbt(ph, "ac%d" % i, [128, D], F32) for i in range(2)]
            ob = [sbt(ph, "ob%d" % i, [128, D], F32) for i in range(2)]
            sm = [sbt(ph, "sm5_%d" % i, [128, 32], F32) for i in range(2)]
            load("sp", l2g[:], l2g_b, ln2g_d.ap())
            load("sp", l2b[:], l2b_b, ln2b_d.ap())
            load("sp", mhalf[:], mhalf_b, cd["mhalf"].ap())
            def fetch5(T):
                ykt, ykb = yk[T % 3]
                xl, xlb = x1l[T % 3]
                for kk in range(4):
                    P.op("pool", lambda e, T=T, kk=kk, ykt=ykt: e.indirect_dma_start(
                        out=ykt[:, kk, :], out_offset=None, in_=YY.ap()[:, :],
                        in_offset=bass.IndirectOffsetOnAxis(ap=idx_all[:, T, kk:kk + 1], axis=0)),
                        reads=(idx_b,), writes=(ykb,), dma=ykb, disjoint=(kk > 0))
                load("sp", xl[:], xlb, X1.ap()[T * 128:(T + 1) * 128, :])

            def comp5(T):
                ykt, ykb = yk[T % 3]
                xl, xlb = x1l[T % 3]
                a, ab_ = ac[T % 2]
                o, ob_ = ob[T % 2]
                smt, smb = sm[T % 2]
                P.op("act", lambda e, T=T, ykt=ykt, a=a: e.activation(out=a[:], in_=ykt[:, 0, :], func=AF.Identity, scale=gates_all[:, T, 0:1]),
                     reads=(ykb, gates_b), writes=(ab_,))
                for kk in range(1, 4):
                    P.op("dve", lambda e, T=T, kk=kk, ykt=ykt, a=a: e.scalar_tensor_tensor(out=a[:], in0=ykt[:, kk, :], scalar=gates_all[:, T, kk:kk + 1], in1=a[:],
                                                                                       op0=ALU.mult, op1=ALU.add), reads=(ykb, gates_b, ab_), writes=(ab_,))
                P.op("dve", lambda e, xl=xl, a=a: e.scalar_tensor_tensor(out=a[:], in0=xl[:], scalar=ALPHA, in1=a[:], op0=ALU.mult, op1=ALU.add),
                     reads=(xlb, ab_), writes=(ab_,))
                emit_ln(P, a, ab_, o, ob_, smt, smb, l2g, l2g_b, l2b, l2b_b, mhalf, mhalf_b, use_act=True, aff_eng=("dve", "pool"))
                store("sp", out_d.ap()[T * 128:(T + 1) * 128, :], o[:], ob_)

            for T in range(NT + 2):
                if T < NT:
                    fetch5(T)
                if T >= 2:
                    comp5(T - 2)
            P.barrier()

        pre3 = {}
        phase1()
        ph23 = ExitStack()
        if last_phase >= 2:
            phase2a()
            pre3["wg"] = sbt(ph23, "wg", [128, 8, 2048], BF16)
            pre3["wpa"] = sbt(ph23, "wpa", [128, 4, 1024], BF16)
            pre3["wpb"] = sbt(ph23, "wpb", [128, 4, 1024], BF16)
            pre3["wout"] = sbt(ph23, "wout", [128, 8, 1024], BF16)
            load("pool", pre3["wg"][0][:], pre3["wg"][1], w_in.ap().rearrange("(kc p) n -> p kc n", p=128)[:, :, 3072:5120])
            load("pool", pre3["wpa"][0][:], pre3["wpa"][1], w_pa.ap().rearrange("(kc p) n -> p kc n", p=128))
            load("pool", pre3["wpb"][0][:], pre3["wpb"][1], w_pb.ap().rearrange("(kc p) n -> p kc n", p=128))
            load("pool", pre3["wout"][0][:], pre3["wout"][1], w_out.ap().rearrange("(kc p) n -> p kc n", p=128))
            phase2b()
        if last_phase >= 3:
            phase3()
        ph23.close()
        if last_phase >= 4:
            phase4()
        if last_phase >= 5:
            phase5()
        P.finalize()
    return nc


def emit_ln(P, z, zb, x1, x1b, smt, smb, g, g_b, b, b_b, mhalf, mhalf_b, o=0, use_act=False, aff_eng=("pool", "pool")):
    P.op("dve", lambda e: e.bn_stats(out=smt[:, o:o + 6], in_=z[:, 0:512]), reads=(zb,), writes=(smb,))
    P.op("dve", lambda e: e.bn_stats(out=smt[:, o + 6:o + 12], in_=z[:, 512:1024]), reads=(zb, smb), writes=(smb,))
    P.op("dve", lambda e: e.bn_aggr(out=smt[:, o + 12:o + 14], in_=smt[:, o:o + 12]), reads=(smb,), writes=(smb,))
    P.op("dve", lambda e: e.tensor_scalar(out=smt[:, o + 14:o + 15], in0=smt[:, o + 13:o + 14], scalar1=LN_EPS, scalar2=None, op0=ALU.add),
         reads=(smb,), writes=(smb,))
    P.op("pool", lambda e: e.tensor_tensor(out=smt[:, o + 15:o + 16], in0=smt[:, o + 14:o + 15], in1=mhalf[:, 0:1], op=ALU.pow),
         reads=(smb, mhalf_b), writes=(smb,))
    if use_act:
        P.op("dve", lambda e: e.tensor_scalar(out=smt[:, o + 16:o + 17], in0=smt[:, o + 12:o + 13], scalar1=smt[:, o + 15:o + 16], scalar2=-1.0,
                                              op0=ALU.mult, op1=ALU.mult), reads=(smb,), writes=(smb,))
        P.op("act", lambda e: e.activation(out=x1[:], in_=z[:], func=AF.Identity, bias=smt[:, o + 16:o + 17], scale=smt[:, o + 15:o + 16]),
             reads=(zb, smb), writes=(x1b,))
    else:
        P.op("dve", lambda e: e.tensor_scalar(out=x1[:], in0=z[:], scalar1=smt[:, o + 12:o + 13], scalar2=smt[:, o + 15:o + 16],
                                              op0=ALU.subtract, op1=ALU.mult), reads=(zb, smb), writes=(x1b,))
    P.op(aff_eng[0], lambda e: e.tensor_tensor(out=x1[:], in0=x1[:], in1=g[:], op=ALU.mult), reads=(x1b, g_b), writes=(x1b,))
    P.op(aff_eng[1], lambda e: e.tensor_tensor(out=x1[:], in0=x1[:], in1=b[:], op=ALU.add), reads=(x1b, b_b), writes=(x1b,))


_NC_CACHE = {}


def prep_shared(inp):
    f = lambda a: np.ascontiguousarray(np.asarray(a, dtype=np.float32))
    b_in = f(inp["b_in"])[0]
    sh = {}
    sh["w_in"] = f(inp["w_in"])[0]
    sh["w_proj_a"] = f(inp["w_proj_a"])[0]
    sh["w_proj_b"] = f(inp["w_proj_b"])[0]
    sh["w_out"] = f(inp["w_out"])[0]
    sh["w_router"] = f(inp["w_router"])[0]
    sh["w_up"] = f(inp["w_up"])[0]
    sh["w_down"] = f(inp["w_down"])[0]
    sh["b_down"] = f(inp["b_down"])[0]
    qkcols = np.concatenate([np.arange(0, 512), np.arange(512, 1024), np.arange(1536, 2048), np.arange(2048, 2560)])
    sh["bqk"] = np.ascontiguousarray(b_in[qkcols].reshape(16, 128).T)
    bv = np.concatenate([b_in[1024:1536], b_in[2560:3072]])
    sh["bv_bc"] = np.ascontiguousarray(np.tile(bv[None, :], (128, 1)))
    sh["bg"] = np.ascontiguousarray(b_in[3072:5120].reshape(16, 128).T)
    bc = lambda v: np.ascontiguousarray(np.tile(f(v).reshape(1, -1), (128, 1)))
    sh["bout_bc"] = bc(inp["b_out"][0])
    sh["ln1g_bc"] = bc(inp["ln1_g"][0])
    sh["ln1b_bc"] = bc(inp["ln1_b"][0])
    sh["ln2g_bc"] = bc(inp["ln2_g"][0])
    sh["ln2b_bc"] = bc(inp["ln2_b"][0])
    sh["subg_bc"] = bc(inp["subln_g"][0])
    lam = np.concatenate([f(inp["lambda_q1"])[0], f(inp["lambda_k1"])[0], f(inp["lambda_q2"])[0], f(inp["lambda_k2"])[0]])
    sh["lam_bc"] = bc(lam)
    sh["brouter_bc"] = bc(inp["b_router"][0])
    bup = f(inp["b_up"])[0]
    sh["b_upT"] = np.ascontiguousarray(bup.reshape(NE, 16, 128).transpose(2, 0, 1).reshape(128, NE * 16))
    for k, v in host_consts().items():
        sh["c_" + k] = np.ascontiguousarray(v)
    return sh


def kernel(**inputs):
    x = np.asarray(inputs["x"], dtype=np.float32)
    sh = prep_shared(inputs)
    if "nc" not in _NC_CACHE:
        _NC_CACHE["nc"] = build()
    nc = _NC_CACHE["nc"]
    in_maps = []
    for c in range(8):
        m = dict(sh)
        m["x"] = np.ascontiguousarray(x[c])
        m["xT"] = np.ascontiguousarray(x[c].T)
        in_maps.append(m)
    res = run_bass_kernel_spmd(nc, in_maps, core_ids=list(range(8)))
    out = np.stack([np.asarray(r["out"], dtype=np.float32) for r in res.results], axis=0)
    return out
```
